# Optimizing a Trainium2 kernel written in Bass

```python
import math
import jax
import jax.numpy as jnp
from jax import lax
import numpy as np

D_MODEL = 2048
BATCH = 2
SEQ = 4096
DEPTH = 1

CHUNK = 64
M_HEADS = 8
M_QK = D_MODEL // 16
M_V = D_MODEL // 8
M_QK_WIDTH = M_HEADS * M_QK
M_WIDTH = M_HEADS * M_V
GATE_CAP = 15.0
G_QK_HEADS = 16
G_V_HEADS = 32
G_HEAD = 128
G_QK_WIDTH = G_QK_HEADS * G_HEAD
G_V_WIDTH = G_V_HEADS * G_HEAD
G_QKV_WIDTH = 2 * G_QK_WIDTH + G_V_WIDTH
CONV_W = 4
N_EXPERTS = 32
TOP_K = 4
D_FF = D_MODEL
SWIGLU_LIMIT = 7.0
SWIGLU_ALPHA = 1.702
EXPERT_BLOCK = 128
LN_EPS = 1e-5
RMS_EPS = 1e-6
DEEPNORM_ALPHA = (2 * DEPTH) ** 0.25
DEEPNORM_BETA = (8 * DEPTH) ** -0.25
SPLIT_SIZES = (M_QK_WIDTH, M_QK_WIDTH, M_WIDTH, M_HEADS, M_HEADS, M_WIDTH,
               G_QKV_WIDTH, G_V_HEADS, G_V_HEADS, G_V_WIDTH,
               D_MODEL, D_MODEL)
IN_WIDTH = sum(SPLIT_SIZES)

kernel_name = 'hybrid_mlstm_gdn_moe_deepnorm'


def layer_norm(x):
    xf = x.astype(jnp.float32)
    mu = xf.mean(-1, keepdims=True)
    var = jnp.square(xf - mu).mean(-1, keepdims=True)
    return (xf - mu) * lax.rsqrt(var + LN_EPS)


def rms_norm(x, w):
    xf = x.astype(jnp.float32)
    return xf * lax.rsqrt(jnp.square(xf).mean(-1, keepdims=True) + RMS_EPS) * w.astype(jnp.float32)


def l2_norm(x):
    xf = x.astype(jnp.float32)
    return xf * lax.rsqrt(jnp.square(xf).sum(-1, keepdims=True) + RMS_EPS)


def to_chunks(t):
    B, S, H = t.shape[:3]
    t = t.reshape((B, S // CHUNK, CHUNK, H) + t.shape[3:])
    return t.transpose((1, 0, 3, 2) + tuple(range(4, t.ndim)))


def from_chunks(t):
    NC, B, H, L = t.shape[:4]
    t = t.transpose((1, 0, 3, 2) + tuple(range(4, t.ndim)))
    return t.reshape((B, NC * L, H) + t.shape[4:])


def causal_depthwise_conv(x, w):
    C = x.shape[-1]
    return lax.conv_general_dilated(
        x, w[:, None, :].astype(x.dtype), window_strides=(1,), padding=[(CONV_W - 1, 0)],
        dimension_numbers=('NWC', 'WIO', 'NWC'), feature_group_count=C)


def mlstm_chunkwise(q, k, v, i_pre, f_pre):
    f32 = jnp.float32
    B, S, H, _ = q.shape
    q = to_chunks(q.astype(f32))
    k = to_chunks(k.astype(f32) * (M_QK ** -0.5))
    v = to_chunks(v.astype(f32))
    i_log = to_chunks(GATE_CAP * jnp.tanh(i_pre.astype(f32) / GATE_CAP))
    f_log = to_chunks(jax.nn.log_sigmoid(f_pre.astype(f32)))
    causal = jnp.tril(jnp.ones((CHUNK, CHUNK), dtype=bool))

    def step(carry, xs):
        C, n, m = carry
        qc, kc, vc, ic, fc = xs
        b = jnp.cumsum(fc, axis=-1)
        dlog = jnp.where(causal, b[..., :, None] - b[..., None, :] + ic[..., None, :], -jnp.inf)
        inter_log = b + m[..., None]
        m_t = jnp.maximum(inter_log, dlog.max(-1))
        s = jnp.einsum('bhtd,bhsd->bhts', qc, kc) * jnp.exp(dlog - m_t[..., None])
        inter = jnp.exp(inter_log - m_t)
        num = inter[..., None] * jnp.einsum('bhtd,bhde->bhte', qc, C) + jnp.einsum('bhts,bhse->bhte', s, vc)
        den = inter * jnp.einsum('bhtd,bhd->bht', qc, n) + s.sum(-1)
        h = num / jnp.maximum(jnp.abs(den), jnp.exp(-m_t))[..., None]
        b_last = b[..., -1]
        state_log = b_last[..., None] - b + ic
        m_new = jnp.maximum(b_last + m, state_log.max(-1))
        decay = jnp.exp(b_last + m - m_new)
        ws = jnp.exp(state_log - m_new[..., None])
        C_new = decay[..., None, None] * C + jnp.einsum('bhsd,bhse->bhde', kc * ws[..., None], vc)
        n_new = decay[..., None] * n + jnp.einsum('bhsd,bhs->bhd', kc, ws)
        return (C_new, n_new, m_new), h

    init = (jnp.zeros((B, H, M_QK, M_V), f32), jnp.zeros((B, H, M_QK), f32), jnp.zeros((B, H), f32))
    _, h = lax.scan(step, init, (q, k, v, i_log, f_log))
    return from_chunks(h)


def gated_delta_chunkwise(q, k, v, g, beta):
    f32 = jnp.float32
    B, S, H, dk = q.shape
    dv = v.shape[-1]
    q, k, v = to_chunks(q.astype(f32)), to_chunks(k.astype(f32)), to_chunks(v.astype(f32))
    g, beta = to_chunks(g.astype(f32)), to_chunks(beta.astype(f32))
    G = jnp.cumsum(g, axis=-1)
    tril = jnp.tril(jnp.ones((CHUNK, CHUNK), dtype=bool))
    strict = jnp.tril(jnp.ones((CHUNK, CHUNK), dtype=bool), -1)
    eye = jnp.eye(CHUNK, dtype=f32)
    decay = jnp.exp(jnp.where(tril, G[..., :, None] - G[..., None, :], -jnp.inf))
    kb = k * beta[..., None]
    a_mat = jnp.where(strict, jnp.einsum('nbhid,nbhjd->nbhij', kb, k) * decay, 0.0) + eye
    rhs = jnp.concatenate([v * beta[..., None], kb * jnp.exp(G)[..., None]], axis=-1)
    sol = lax.linalg.triangular_solve(a_mat, rhs, left_side=True, lower=True, unit_diagonal=True)
    u, w = sol[..., :dv], sol[..., dv:]
    attn = jnp.einsum('nbhid,nbhjd->nbhij', q, k) * decay
    q_dec = q * jnp.exp(G)[..., None]
    k_dec = k * jnp.exp(G[..., -1:] - G)[..., None]
    g_last = jnp.exp(G[..., -1])

    def step(state, xs):
        attn_c, u_c, w_c, qd_c, kd_c, gl_c = xs
        v_new = u_c - jnp.einsum('bhld,bhde->bhle', w_c, state)
        o = jnp.einsum('bhld,bhde->bhle', qd_c, state) + jnp.einsum('bhls,bhse->bhle', attn_c, v_new)
        state = state * gl_c[..., None, None] + jnp.einsum('bhld,bhle->bhde', kd_c, v_new)
        return state, o

    _, o = lax.scan(step, jnp.zeros((B, H, dk, dv), f32), (attn, u, w, q_dec, k_dec, g_last))
    return from_chunks(o)


def moe_ffn(h, w_router, b_router, w_up, b_up, w_down, b_down):
    T, D = h.shape
    logits = (jnp.dot(h, w_router) + b_router).astype(jnp.float32)
    top_v, top_e = lax.top_k(logits, TOP_K)
    gate = jax.nn.softmax(top_v, axis=-1)
    n_slots = T * TOP_K
    flat_e = top_e.reshape(-1)
    order = jnp.argsort(flat_e)
    sorted_e = flat_e[order]
    counts = jnp.bincount(flat_e, length=N_EXPERTS)
    padded = (counts + EXPERT_BLOCK - 1) // EXPERT_BLOCK * EXPERT_BLOCK
    pad_end = jnp.cumsum(padded)
    pad_start = pad_end - padded
    start = jnp.cumsum(counts) - counts
    dest = pad_start[sorted_e] + jnp.arange(n_slots) - start[sorted_e]
    n_buf = n_slots + N_EXPERTS * EXPERT_BLOCK
    n_blocks = n_buf // EXPERT_BLOCK
    tok = order // TOP_K
    buf = jnp.zeros((n_buf, D), h.dtype).at[dest].set(h[tok])
    block_e = jnp.minimum(jnp.searchsorted(pad_end, jnp.arange(n_blocks) * EXPERT_BLOCK, side='right'),
                          N_EXPERTS - 1)

    def expert_block(args):
        xb, e = args
        hu = jnp.dot(xb, w_up[e]) + b_up[e]
        g_lin = jnp.minimum(hu[:, 0::2], SWIGLU_LIMIT)
        up = jnp.clip(hu[:, 1::2], -SWIGLU_LIMIT, SWIGLU_LIMIT)
        act = (up + 1.0) * g_lin * jax.nn.sigmoid(SWIGLU_ALPHA * g_lin)
        return jnp.dot(act, w_down[e]) + b_down[e]

    out_buf = lax.map(expert_block, (buf.reshape(n_blocks, EXPERT_BLOCK, D), block_e)).reshape(n_buf, D)
    slot_out = out_buf[dest] * gate.reshape(-1)[order][:, None].astype(h.dtype)
    return jnp.zeros((T, D), h.dtype).at[tok].add(slot_out)


def hybrid_layer(x, c, w_ada, b_ada, w_in, m_bias_i, m_bias_f, m_norm_w, conv_w, g_a_log, g_dt_bias,
                 g_norm_w, w_branch_a, w_branch_b, w_out, ln1_g, ln1_b, w_router, b_router,
                 w_up, b_up, w_down, b_down, ln2_g, ln2_b):
    B, S, D = x.shape
    dt = x.dtype
    f32 = jnp.float32
    mod = jnp.dot(jax.nn.silu(c), w_ada) + b_ada
    shift1, scale1, gate1, shift2, scale2, gate2 = jnp.split(mod[:, None, :], 6, axis=-1)

    h = (layer_norm(x) * (1.0 + scale1) + shift1).astype(dt)
    proj = jnp.dot(h, w_in)
    split_at = tuple(int(p) for p in np.cumsum(SPLIT_SIZES)[:-1])
    mq, mk, mv, mi, mf, mo, g_qkv, ga, gb, gz, ra, rb = jnp.split(proj, split_at, axis=-1)

    hm = mlstm_chunkwise(mq.reshape(B, S, M_HEADS, M_QK), mk.reshape(B, S, M_HEADS, M_QK),
                         mv.reshape(B, S, M_HEADS, M_V), mi + m_bias_i, mf + m_bias_f)
    hm = rms_norm(hm, m_norm_w.reshape(M_HEADS, M_V)) * jax.nn.sigmoid(mo.astype(f32)).reshape(B, S, M_HEADS, M_V)
    y_a = jnp.dot(hm.reshape(B, S, M_WIDTH).astype(dt), w_branch_a)

    g_qkv = jax.nn.silu(causal_depthwise_conv(g_qkv, conv_w))
    gq, gk, gv = jnp.split(g_qkv, (G_QK_WIDTH, 2 * G_QK_WIDTH), axis=-1)
    rep = G_V_HEADS // G_QK_HEADS
    q = jnp.repeat(l2_norm(gq.reshape(B, S, G_QK_HEADS, G_HEAD)) * (G_HEAD ** -0.5), rep, axis=2)
    k = jnp.repeat(l2_norm(gk.reshape(B, S, G_QK_HEADS, G_HEAD)), rep, axis=2)
    v = gv.reshape(B, S, G_V_HEADS, G_HEAD)
    beta = jax.nn.sigmoid(gb.astype(f32))
    g = -jnp.exp(g_a_log.astype(f32)) * jax.nn.softplus(ga.astype(f32) + g_dt_bias.astype(f32))
    o = gated_delta_chunkwise(q, k, v, g, beta)
    o = rms_norm(o, g_norm_w) * jax.nn.silu(gz.astype(f32)).reshape(B, S, G_V_HEADS, G_HEAD)
    y_b = jnp.dot(o.reshape(B, S, G_V_WIDTH).astype(dt), w_branch_b)

    merged = jax.nn.sigmoid(ra) * y_a + jax.nn.sigmoid(rb) * y_b
    mix = jnp.dot(merged, w_out)
    x = (layer_norm(DEEPNORM_ALPHA * x + gate1 * mix) * ln1_g + ln1_b).astype(dt)

    h2 = (layer_norm(x) * (1.0 + scale2) + shift2).astype(dt)
    ffn = moe_ffn(h2.reshape(B * S, D), w_router, b_router, w_up, b_up, w_down, b_down).reshape(B, S, D)
    x = (layer_norm(DEEPNORM_ALPHA * x + gate2 * ffn) * ln2_g + ln2_b).astype(dt)
    return x


def setup_inputs(seed: int = 0) -> dict:
    key = jax.random.key(seed)
    ks = jax.random.split(key, 26)
    f32 = jnp.float32
    L = DEPTH

    def normal(k, shape, std):
        return jax.random.normal(k, shape, f32) * std

    def near_one(k, shape):
        return 1.0 + 0.02 * jax.random.normal(k, shape, f32)

    x = normal(ks[0], (BATCH, SEQ, D_MODEL), 1.0)
    c = normal(ks[1], (BATCH, D_MODEL), 1.0)
    w_ada = normal(ks[2], (L, D_MODEL, 6 * D_MODEL), 0.5 * D_MODEL ** -0.5)
    b_ada = normal(ks[3], (L, 6 * D_MODEL), 0.02)
    w_in = normal(ks[4], (L, D_MODEL, IN_WIDTH), D_MODEL ** -0.5)
    m_bias_i = normal(ks[5], (L, M_HEADS), 0.1)
    m_bias_f = jnp.linspace(3.0, 6.0, M_HEADS, dtype=f32)[None, :] + normal(ks[6], (L, M_HEADS), 0.1)
    m_norm_w = near_one(ks[7], (L, M_WIDTH))
    conv_w = normal(ks[8], (L, CONV_W, G_QKV_WIDTH), CONV_W ** -0.5)
    g_a_log = jnp.log(jax.random.uniform(ks[9], (L, G_V_HEADS), f32, 1.0, 16.0))
    dt_init = jnp.exp(jax.random.uniform(ks[10], (L, G_V_HEADS), f32, math.log(1e-3), math.log(1e-1)))
    g_dt_bias = dt_init + jnp.log(-jnp.expm1(-dt_init))
    g_norm_w = near_one(ks[11], (L, G_HEAD))
    w_branch_a = normal(ks[12], (L, M_WIDTH, D_MODEL), M_WIDTH ** -0.5)
    w_branch_b = normal(ks[13], (L, G_V_WIDTH, D_MODEL), G_V_WIDTH ** -0.5)
    w_out = normal(ks[14], (L, D_MODEL, D_MODEL), DEEPNORM_BETA * D_MODEL ** -0.5)
    ln1_g = near_one(ks[15], (L, D_MODEL))
    ln1_b = normal(ks[16], (L, D_MODEL), 0.02)
    w_router = normal(ks[17], (L, D_MODEL, N_EXPERTS), D_MODEL ** -0.5)
    b_router = normal(ks[18], (L, N_EXPERTS), 0.01)
    w_up = normal(ks[19], (L, N_EXPERTS, D_MODEL, 2 * D_FF), D_MODEL ** -0.5)
    b_up = normal(ks[20], (L, N_EXPERTS, 2 * D_FF), 0.02)
    w_down = normal(ks[21], (L, N_EXPERTS, D_FF, D_MODEL), DEEPNORM_BETA * D_FF ** -0.5)
    b_down = normal(ks[22], (L, N_EXPERTS, D_MODEL), 0.02)
    ln2_g = near_one(ks[23], (L, D_MODEL))
    ln2_b = normal(ks[24], (L, D_MODEL), 0.02)
    return {'x': x, 'c': c, 'w_ada': w_ada, 'b_ada': b_ada, 'w_in': w_in,
            'm_bias_i': m_bias_i, 'm_bias_f': m_bias_f, 'm_norm_w': m_norm_w,
            'conv_w': conv_w, 'g_a_log': g_a_log, 'g_dt_bias': g_dt_bias, 'g_norm_w': g_norm_w,
            'w_branch_a': w_branch_a, 'w_branch_b': w_branch_b, 'w_out': w_out,
            'ln1_g': ln1_g, 'ln1_b': ln1_b, 'w_router': w_router, 'b_router': b_router,
            'w_up': w_up, 'b_up': b_up, 'w_down': w_down, 'b_down': b_down,
            'ln2_g': ln2_g, 'ln2_b': ln2_b}


def reference(x, c, w_ada, b_ada, w_in, m_bias_i, m_bias_f, m_norm_w, conv_w, g_a_log, g_dt_bias,
              g_norm_w, w_branch_a, w_branch_b, w_out, ln1_g, ln1_b, w_router, b_router,
              w_up, b_up, w_down, b_down, ln2_g, ln2_b):
    for l in range(DEPTH):
        x = hybrid_layer(x, c, w_ada[l], b_ada[l], w_in[l], m_bias_i[l], m_bias_f[l], m_norm_w[l],
                         conv_w[l], g_a_log[l], g_dt_bias[l], g_norm_w[l], w_branch_a[l], w_branch_b[l],
                         w_out[l], ln1_g[l], ln1_b[l], w_router[l], b_router[l], w_up[l], b_up[l],
                         w_down[l], b_down[l], ln2_g[l], ln2_b[l])
    return x
```

```python
import numpy as np
import concourse.bass as bass
import concourse.mybir as mybir
from concourse.bass_utils import run_bass_kernel_spmd

F32 = mybir.dt.float32
BF16 = mybir.dt.bfloat16
AF = mybir.ActivationFunctionType
ALU = mybir.AluOpType

SEM_PHASE = 12000
DMA_POOL = 24

D = 2048
DC = 16
IN_W = 22608
LN_EPS = 1e-5
RMS_EPS = 1e-6
ALPHA = 2.0 ** 0.25
NCONST = 11 * 128


class Ins:
    __slots__ = ("eng", "fn", "reads", "writes", "dma", "idx", "deps", "sig", "semv", "name")


class Prog:
    def __init__(self, nc):
        self.nc = nc
        self.ins = []
        self.tinfo = {}
        self.engs = {"pe": nc.tensor, "act": nc.scalar, "dve": nc.vector, "pool": nc.gpsimd, "sp": nc.sync}
        self._ctx = []
        self.self_sync = {"dve", "act", "pool"}
        self.uid = 0

    def sbuf(self, name, shape, dtype):
        g = self.nc.sbuf_tensor(name, list(shape), dtype)
        t = g.__enter__()
        self._ctx.append(g)
        self.tinfo[t.name] = (int(np.prod(shape[1:])) * mybir.dt.size(dtype), "sb")
        return t

    def psum(self, name, shape, dtype):
        g = self.nc.psum_tensor(name, list(shape), dtype)
        t = g.__enter__()
        self._ctx.append(g)
        self.tinfo[t.name] = (int(np.prod(shape[1:])) * mybir.dt.size(dtype), "ps")
        return t

    def dram(self, name, shape, dtype, kind="Internal"):
        t = self.nc.dram_tensor(name, list(shape), dtype, kind=kind)
        self.tinfo[t.name] = (None, "dr")
        return t

    def mark(self):
        return len(self._ctx)

    def release(self, mark):
        self.barrier()
        while len(self._ctx) > mark:
            g = self._ctx.pop()
            g.__exit__(None, None, None)

    def uniq(self, tag):
        self.uid += 1
        return (tag, 0, 1, self.uid, self.uid + 1)

    def region(self, ap):
        name = ap.tensor.name
        info = self.tinfo.get(name)
        if info is None:
            return None
        fe, sp = info
        pat = ap.ap
        es = mybir.dt.size(ap.dtype)
        off = int(ap.offset) * es
        if sp == "dr":
            ext = es
            for st, cnt in pat:
                ext += (cnt - 1) * abs(st) * es
            return (name, 0, 1, off, off + ext)
        if sp == "ps":
            return (name, 0, 128, 0, fe)
        pst, pcnt = pat[0]
        pst *= es
        p0 = off // fe
        f0 = off % fe
        pstep = max(1, pst // fe) if pst else 0
        p1 = p0 + (pcnt - 1) * pstep + 1
        ext = es
        for st, cnt in pat[1:]:
            ext += (cnt - 1) * abs(st) * es
        return (name, p0, p1, f0, f0 + ext)

    def add(self, eng, fn, reads=(), writes=(), dma=False, name=""):
        i = Ins()
        i.eng = eng
        i.fn = fn
        i.reads = [r for r in (a if isinstance(a, tuple) else self.region(a) for a in reads) if r is not None]
        i.writes = [r for r in (a if isinstance(a, tuple) else self.region(a) for a in writes) if r is not None]
        i.dma = dma
        i.idx = len(self.ins)
        i.deps = set()
        i.sig = False
        i.semv = None
        i.name = name
        self.ins.append(i)
        return i

    def barrier(self):
        self.add("sp", lambda: self.nc.sync.nop(), writes=[("BAR", 0, 1, 0, 1)], name="bar1")
        for e in ("pe", "act", "dve", "pool"):
            self.add(e, lambda: None, reads=[("BAR", 0, 1, 0, 1)], name="bar2")

    def mm(self, out, lhsT, rhs, start=True, stop=True):
        return self.add("pe", lambda: self.nc.tensor.matmul(out, lhsT, rhs, start=start, stop=stop),
                        reads=[lhsT, rhs] + ([] if start else [out]), writes=[out])

    def tr(self, out, in_, ident):
        return self.add("pe", lambda: self.nc.tensor.transpose(out, in_, ident), reads=[in_, ident], writes=[out])

    def act(self, out, in_, func, bias=None, scale=None, accum_out=None):
        kw = {}
        rd = [in_]
        if bias is not None:
            kw["bias"] = bias
            if not isinstance(bias, (int, float)):
                rd.append(bias)
        if scale is not None:
            kw["scale"] = scale
            if not isinstance(scale, (int, float)):
                rd.append(scale)
        wr = [out]
        if accum_out is not None:
            kw["accum_out"] = accum_out
            wr.append(accum_out)
        return self.add("act", lambda: self.nc.scalar.activation(out, in_, func, **kw), reads=rd, writes=wr)

    def _veng(self, eng):
        return self.nc.vector if eng == "dve" else self.nc.gpsimd

    def ts(self, out, in0, s1, s2, op0, op1=None, eng="dve", accum_out=None):
        rd = [in0] + [s for s in (s1, s2) if s is not None and not isinstance(s, (int, float))]
        kw = {}
        wr = [out]
        if accum_out is not None:
            kw["accum_out"] = accum_out
            wr.append(accum_out)
        if op1 is None:
            return self.add(eng, lambda: self._veng(eng).tensor_scalar(out, in0, s1, None, op0, **kw), reads=rd, writes=wr)
        return self.add(eng, lambda: self._veng(eng).tensor_scalar(out, in0, s1, s2, op0, op1, **kw), reads=rd, writes=wr)

    def tt(self, out, in0, in1, op, eng="dve"):
        return self.add(eng, lambda: self._veng(eng).tensor_tensor(out, in0, in1, op), reads=[in0, in1], writes=[out])

    def stt(self, out, in0, scalar, in1, op0, op1):
        rd = [in0, in1] + ([] if isinstance(scalar, (int, float)) else [scalar])
        return self.add("dve", lambda: self.nc.vector.scalar_tensor_tensor(out, in0, scalar, in1, op0, op1), reads=rd, writes=[out])

    def copy(self, out, in_, eng="dve"):
        if eng == "act":
            return self.add("act", lambda: self.nc.scalar.copy(out, in_), reads=[in_], writes=[out])
        return self.add(eng, lambda: self._veng(eng).tensor_copy(out, in_), reads=[in_], writes=[out])

    def memset(self, out, val, eng="dve"):
        return self.add(eng, lambda: self._veng(eng).memset(out, val), reads=[], writes=[out])

    def recip(self, out, in_):
        return self.add("dve", lambda: self.nc.vector.reciprocal(out, in_), reads=[in_], writes=[out])

    def dma(self, out, in_, eng="sp", rd=None, wr=None, **kw):
        return self.add(eng, lambda: self.engs[eng].dma_start(out, in_, **kw),
                        reads=[in_] if rd is None else rd, writes=[out] if wr is None else wr, dma=True)

    def emit(self):
        nc = self.nc
        recs = {}
        last_on = {}
        dmas_open = []

        def ovl(r, q):
            return r[0] < q[2] and q[1] < r[1] and r[2] < q[4] and q[3] < r[3]

        for ins in self.ins:
            deps = ins.deps
            if ins.name == "bar1":
                deps.update(last_on.values())
                deps.update(dmas_open)
                dmas_open = []
                recs = {}
            for q in ins.reads:
                isps = self.tinfo.get(q[0], (0, ""))[1] == "ps"
                for r in recs.get(q[0], ()):
                    if ovl(r, q):
                        if r[4] is not None:
                            deps.add(r[4])
                        if isps:
                            deps.update(j for e_, j in r[5].items() if e_ != ins.eng)
                        if ins.dma:
                            r[6].append(ins.idx)
                        else:
                            r[5][ins.eng] = ins.idx
            for q in ins.writes:
                lst = recs.get(q[0], ())
                keep = []
                for r in lst:
                    if ovl(r, q):
                        if r[4] is not None:
                            deps.add(r[4])
                        deps.update(r[5].values())
                        deps.update(r[6])
                        if q[1] <= r[0] and r[1] <= q[2] and q[3] <= r[2] and r[3] <= q[4]:
                            continue
                    keep.append(r)
                keep.append([q[1], q[2], q[3], q[4], ins.idx, {}, []])
                recs[q[0]] = keep
            deps.discard(ins.idx)
            if ins.dma:
                dmas_open.append(ins.idx)
            elif ins.name not in ("bar1", "bar2"):
                last_on[ins.eng] = ins.idx

        for ins in self.ins:
            nd = set()
            for j in ins.deps:
                p = self.ins[j]
                if p.eng == ins.eng and not p.dma and (ins.eng not in self.self_sync):
                    continue
                nd.add(j)
            ins.deps = nd
            for j in nd:
                self.ins[j].sig = True

        self._sems = []

        def newsem(nm):
            g = nc.semaphore(nm)
            s = g.__enter__()
            self._sems.append(g)
            return s

        eng_sems = {e: [] for e in self.engs}
        eng_cnt = {e: 0 for e in self.engs}
        dma_sems = [newsem(f"dq{k}") for k in range(DMA_POOL)]
        dma_n = 0
        known = {e: {} for e in self.engs}

        def wait(engname, sem, val):
            k = known[engname]
            key = id(sem)
            if k.get(key, 0) >= val:
                return
            k[key] = val
            self.engs[engname].wait_ge(sem, val)

        for ins in self.ins:
            e = ins.eng
            for j in sorted(ins.deps):
                p = self.ins[j]
                if p.semv is None:
                    raise RuntimeError(f"dep on unsignaled instr {p.name} {p.eng}")
                wait(e, p.semv[0], p.semv[1])
            if ins.dma:
                slot = dma_n % DMA_POOL
                val = 16 * (dma_n // DMA_POOL + 1)
                if val > 16:
                    wait(e, dma_sems[slot], val - 16)
                dma_n += 1
                ins.semv = (dma_sems[slot], val)
                ins.fn().then_inc(dma_sems[slot], 16)
            else:
                bi = ins.fn()
                if ins.sig:
                    if bi is None:
                        raise RuntimeError("signal needed on empty instr")
                    c = eng_cnt[e]
                    ph = c // SEM_PHASE
                    while len(eng_sems[e]) <= ph:
                        eng_sems[e].append(newsem(f"s_{e}{len(eng_sems[e])}"))
                    sem = eng_sems[e][ph]
                    eng_cnt[e] = c + 1
                    ins.semv = (sem, c % SEM_PHASE + 1)
                    bi.then_inc(sem, 1)
        self.n_dma = dma_n
        self.counts = dict(eng_cnt)


def make_consts():
    i = np.arange(128)
    ident = np.eye(128, dtype=np.float32)
    U = (i[:, None] <= i[None, :]).astype(np.float32)
    GT = (i[:, None] > i[None, :]).astype(np.float32)
    lv = []
    for l in range(7):
        b = 1 << l
        m = ((i[:, None] // (2 * b) == i[None, :] // (2 * b)) & ((i[:, None] // b) % 2 == 1)
             & ((i[None, :] // b) % 2 == 0))
        lv.append(-m.astype(np.float32))
    ones = np.ones((128, 128), np.float32)
    return np.concatenate([ident, U, GT] + lv + [ones], axis=1)


class Builder:
    def __init__(self, S, NE, debug=False, phases="0ABC"):
        self.S = S
        self.NE = NE
        self.NT = S // 128
        self.debug = debug
        self.phases = phases
        nc = bass.Bass("TRN2", target_bir_lowering=False)
        self.nc = nc
        self.P = Prog(nc)
        self.decl = set()
        self.build()

    def din(self, name, shape, dtype=F32, ph="0ABC"):
        if not any(p in self.phases for p in ph):
            return None
        self.decl.add(name)
        return self.nc.dram_tensor(name, list(shape), dtype, kind="ExternalInput").ap()

    def scratch(self, name, shape, dtype):
        kind = "ExternalOutput" if self.debug else "Internal"
        t = self.nc.dram_tensor(name, list(shape), dtype, kind=kind)
        self.P.tinfo[t.name] = (None, "dr")
        return t.ap()

    def build(self):
        P, nc, S, NE = self.P, self.nc, self.S, self.NE
        self.x = self.din("x", [S, D])
        self.c_in = self.din("c", [128, 16])
        self.w_ada = self.din("w_ada", [D, 6 * D], ph="0")
        self.b_ada = self.din("b_ada", [1, 6 * D], ph="0")
        self.w_in_m = self.din("w_in_m", [8, 128, 16 * 770], ph="A")
        self.w_in_g = self.din("w_in_g", [16, 128, 16 * 772], ph="A")
        self.w_rarb = self.din("w_rarb", [16, 128, 4096], ph="B")
        self.m_bias = self.din("m_bias", [1, 16], ph="A")
        self.m_norm_w = self.din("m_norm_w", [1, D], ph="A")
        self.conv_g = self.din("conv_g", [16, 128, 16], ph="A")
        self.g_ab = self.din("g_ab", [1, 64], ph="A")
        self.g_norm_w = self.din("g_norm_w", [1, 128], ph="A")
        self.w_a = self.din("w_a", [16, 128, 2048], ph="B")
        self.w_b = self.din("w_b", [16, 128, 4096], ph="B")
        self.w_out = self.din("w_out", [4, 128, 8192], ph="B")
        self.lnp = self.din("lnp", [4, D])
        self.w_router = self.din("w_router", [D, NE], ph="B")
        self.b_router = self.din("b_router", [1, NE], ph="B")
        self.EG = min(8, NE)
        self.w_ups = [self.din(f"w_up{i}", [self.EG, 16, 128, 4096], ph="C") for i in range(NE // self.EG)]
        self.b_upg = self.din("b_upg", [128, NE * 16], ph="C")
        self.b_upu = self.din("b_upu", [128, NE * 16], ph="C")
        self.w_downs = [self.din(f"w_down{i}", [self.EG, 8, 128, 4096], ph="C") for i in range(NE // self.EG)]
        self.b_down = self.din("b_down", [NE, D], ph="C")
        self.consts_in = self.din("consts", [128, NCONST])
        self.out = self.nc.dram_tensor("out", [S, D], F32, kind="ExternalOutput").ap()
        P.tinfo[self.out.tensor.name] = (None, "dr")

        self.modrow = self.scratch("modrow", [1, 6 * D], F32)
        self.hT = self.scratch("hT", [D, S], BF16)
        self.mix = self.scratch("mix", [S, 3 * D], BF16)
        self.pre = self.scratch("pre", [S, D], F32)
        self.x1 = self.scratch("x1", [S, D], F32)
        self.h2T = self.scratch("h2T", [D, S], BF16)
        self.gates_d = self.scratch("gates", [S, NE], F32)
        self.ffn = self.scratch("ffn", [S, D], F32)

        self.cst = P.sbuf("cst", [128, NCONST], F32)
        P.dma(self.cst[:, :], self.consts_in[:, :])
        c = self.cst
        self.ident = c[:, 0:128]
        self.U = c[:, 128:256]
        self.GT = c[:, 256:384]
        self.negm = [c[:, 384 + 128 * l: 512 + 128 * l] for l in range(7)]
        self.ones = c[:, 1280:1408]
        self.identb = P.sbuf("identb", [128, 128], BF16)
        P.copy(self.identb[:, :], self.ident)
        self.bi = 0
        self.qbi = 0
        self.pn = 0

        if "0" in self.phases:
            self.phase0()
        if "A" in self.phases:
            self.phaseA()
        if "B" in self.phases:
            self.phaseB()
        if "C" in self.phases:
            self.phaseC()
        P.add("sp", lambda: None, reads=[self.out[:, :]] if "C" in self.phases else [], writes=[], name="fin")
        P.barrier()
        P.emit()

    def set_psum(self, nf, nb):
        self.pn += 1
        self.big = [self.P.psum(f"pf{self.pn}_{i}", [128, 512], F32) for i in range(nf)]
        self.bfb = [self.P.psum(f"pb{self.pn}_{i}", [128, 1024], BF16) for i in range(nb)]

    def bank(self):
        b = self.big[self.bi % len(self.big)]
        self.bi += 1
        return b

    def q(self, n=128):
        return self.bank()[:, 0:n]

    def ln_stats(self, src, st, mv, rstd, nmr, eps):
        P, nc = self.P, self.nc
        for k in range(4):
            P.add("dve", (lambda k=k: nc.vector.bn_stats(st[:, k * 6:(k + 1) * 6], src[:, k * 512:(k + 1) * 512])),
                  reads=[src[:, k * 512:(k + 1) * 512]], writes=[st[:, k * 6:(k + 1) * 6]])
        P.add("dve", lambda: nc.vector.bn_aggr(mv[:, 0:2], st[:, 0:24]), reads=[st[:, 0:24]], writes=[mv[:, 0:2]])
        P.ts(mv[:, 2:3], mv[:, 1:2], eps, None, ALU.add)
        P.act(mv[:, 3:4], mv[:, 2:3], AF.Sqrt)
        P.recip(rstd, mv[:, 3:4])
        P.ts(nmr, mv[:, 0:1], -1.0, rstd, ALU.mult, ALU.mult)

    def rstd_from_ss(self, ss, n, tmp, rstd):
        P = self.P
        P.ts(tmp, ss, 1.0 / n, RMS_EPS, ALU.mult, ALU.add)
        P.act(tmp, tmp, AF.Sqrt)
        P.recip(rstd, tmp)

    def softplus_parts(self, y, a, e, l):
        P = self.P
        P.stt(a, y, -1.0, y, ALU.mult, ALU.max)
        P.act(e, a, AF.Exp, scale=-1.0)
        P.ts(e, e, 1.0, None, ALU.add)
        P.act(l, e, AF.Ln)

    def phase0(self):
        import os
        BIS = int(os.environ.get("BIS", "255"))
        P, nc, S = self.P, self.nc, self.S
        mk = P.mark()
        self.set_psum(2, 4)
        csb = P.sbuf("csb", [128, 16], F32)
        scs = P.sbuf("scs", [128, 16], F32)
        P.dma(csb[:, :], self.c_in[:, :])
        P.act(scs[:, :], csb[:, :], AF.Silu)
        wt = [P.sbuf(f"wada{i}", [128, 16, 512], F32) for i in range(2)]
        bt = [P.sbuf(f"bada{i}", [1, 512], F32) for i in range(2)]
        row = [P.sbuf(f"mrow{i}", [1, 512], F32) for i in range(2)]
        wv = self.w_ada.rearrange("(p c) n -> p c n", c=16)
        for n in range(24 if BIS & 1 else 0):
            w = wt[n % 2]
            P.dma(w[:, :, :], wv[:, :, n * 512:(n + 1) * 512])
            ps = self.bank()
            for cc in range(16):
                P.mm(ps[0:1, :], scs[:, cc:cc + 1], w[:, cc, :], start=(cc == 0), stop=(cc == 15))
            P.dma(bt[n % 2][:, :], self.b_ada[0:1, n * 512:(n + 1) * 512])
            P.tt(row[n % 2][0:1, :], ps[0:1, :], bt[n % 2][0:1, :], ALU.add)
            P.dma(self.modrow[0:1, n * 512:(n + 1) * 512], row[n % 2][:, :])
        sc1 = P.sbuf("sc1T", [128, 16], F32)
        sh1 = P.sbuf("sh1T", [128, 16], F32)
        if BIS & 2:
            P.dma(sh1[:, :], self.modrow[0, 0:D].rearrange("(c p) -> p c", p=128), allow_slow_non_contiguous=True)
            P.dma(sc1[:, :], self.modrow[0, D:2 * D].rearrange("(c p) -> p c", p=128), allow_slow_non_contiguous=True)
        else:
            P.memset(sh1[:, :], 0.0)
            P.memset(sc1[:, :], 0.0)
        P.ts(sc1[:, :], sc1[:, :], 1.0, None, ALU.add)
        xt = [P.sbuf(f"x0_{i}", [128, D], F32) for i in range(2)]
        xn = [P.sbuf(f"xn0_{i}", [128, D], BF16) for i in range(2)]
        ho = [P.sbuf(f"ho_{i}", [128, 16, 128], BF16) for i in range(2)]
        st = P.sbuf("st0", [128, 24], F32)
        mv = P.sbuf("mv0", [128, 8], F32)
        ptb = [P.psum(f"ptb{i}", [128, 4, 128], BF16) for i in range(2)] if False else None
        hTv = self.hT.rearrange("(c p) s -> p c s", p=128)
        for t in range(self.NT if BIS & 4 else 0):
            x_ = xt[t % 2]
            P.dma(x_[:, :], self.x[t * 128:(t + 1) * 128, :])
            self.ln_stats(x_, st, mv, mv[:, 4:5], mv[:, 5:6], LN_EPS)
            xn_ = xn[t % 2]
            P.act(xn_[:, :], x_[:, :], AF.Identity, bias=mv[:, 5:6], scale=mv[:, 4:5])
            h_ = ho[t % 2]
            for c4 in range(4):
                pb = self.qb()
                for k in range(4):
                    cc = c4 * 4 + k
                    P.tr(pb[:, k * 128:(k + 1) * 128], xn_[:, cc * 128:(cc + 1) * 128], self.identb[:, :])
                for k in range(4):
                    cc = c4 * 4 + k
                    P.ts(h_[:, cc, :], pb[:, k * 128:(k + 1) * 128], sc1[:, cc:cc + 1], sh1[:, cc:cc + 1], ALU.mult, ALU.add)
            if BIS & 16:
                P.dma(hTv[:, :, t * 128:(t + 1) * 128], h_[:, :, :], wr=[P.uniq("hT")])
        P.release(mk)

    def qb(self):
        b = self.bfb[self.qbi % len(self.bfb)]
        self.qbi += 1
        return b

    def phaseA(self):
        P, nc, S = self.P, self.nc, self.S
        mk = P.mark()
        self.set_psum(8, 0)
        NTL = S // 512
        hTv = self.hT.rearrange("(c p) s -> p c s", p=128)
        hbuf = [P.sbuf(f"hA{i}", [128, 16, 512], BF16) for i in range(2)]
        wbuf = [P.sbuf(f"wA{i}", [128, 16, 772], BF16) for i in range(2)]
        mb = P.sbuf("mb", [128, 16], F32)
        P.dma(mb[:, :], self.m_bias[0:1, :].partition_broadcast(128))
        P.ts(mb[:, 0:8], mb[:, 0:8], 1.0 / 15.0, None, ALU.mult)
        gab = P.sbuf("gab", [128, 64], F32)
        P.dma(gab[:, :], self.g_ab[0:1, :].partition_broadcast(128))
        P.act(gab[:, 0:32], gab[:, 0:32], AF.Exp)
        P.ts(gab[:, 0:32], gab[:, 0:32], -1.0, None, ALU.mult)
        gnw = P.sbuf("gnw", [128, 128], F32)
        P.dma(gnw[:, :], self.g_norm_w[0:1, :].partition_broadcast(128))
        mnw = P.sbuf("mnw", [128, 256], F32)
        cw = P.sbuf("cw", [128, 16], F32)

        W = {}

        def wt(name, shape=(128, 128), dt=F32, n=2):
            W[name] = [P.sbuf(f"A_{name}{i}", list(shape), dt) for i in range(n)]

        for nm in ["Fm", "fbc", "DmT", "DmTm", "eb", "SmT", "QdT", "kd", "ktm", "dec", "decT", "eGb", "decmb",
                   "decTm", "attnT", "Nm", "NT", "T", "R", "Y", "tmp", "vb", "kbg", "kdec", "nwT", "vnew", "sz", "t1"]:
            wt(nm)
        wt("v1", (128, 257))
        wt("tk3", (128, 260))
        wt("KK")
        wt("AT")
        wt("hraw", (128, 256))
        wt("sig", (128, 256))
        wt("junk", (128, 256))
        wt("hm", (128, 256), BF16)
        wt("on", (128, 128), BF16)
        wt("col", (128, 16), n=4)
        self.wi = {k: 0 for k in W}

        def g(name):
            i = self.wi[name]
            self.wi[name] = i + 1
            return W[name][i % len(W[name])]

        qT_all = P.sbuf("qT_all", [128, 512], F32)
        kT_all = P.sbuf("kT_all", [128, 512], F32)
        Cn = P.sbuf("Cn", [128, 257], F32)
        for v in W["v1"]:
            P.memset(v[:, 256:257], 1.0)

        load_n = [0]

        def load_h(T):
            hb = hbuf[load_n[0] % 2]
            load_n[0] += 1
            P.dma(hb[:, :, :], hTv[:, :, T * 512:(T + 1) * 512], rd=[P.uniq("hTr")])
            return hb

        for hd in range(8):
            wm = wbuf[hd % 2]
            P.dma(wm[:, :, 0:770], self.w_in_m[hd].rearrange("p (c n) -> p c n", c=16), eng="pool")
            P.dma(mnw[:, :], self.m_norm_w[0:1, hd * 256:(hd + 1) * 256].partition_broadcast(128))
            P.memset(Cn[:, :], 0.0)
            hb_next = load_h(0)
            for T in range(NTL):
                hb = hb_next
                if T + 1 < NTL:
                    hb_next = load_h(T + 1)
                for ct, dst, sc in ((0, qT_all, 1.0), (1, kT_all, 128.0 ** -0.5)):
                    ps = self.bank()
                    for cc in range(16):
                        P.mm(ps[:, :], wm[:, cc, ct * 128:(ct + 1) * 128], hb[:, cc, :], start=(cc == 0), stop=(cc == 15))
                    P.act(dst[:, :], ps[:, :], AF.Copy, scale=sc)
                for j in range(4):
                    tok0 = T * 512 + j * 128
                    ps1 = self.bank()
                    ps2 = self.bank()
                    for cc in range(16):
                        P.mm(ps1[:, 0:386], hb[:, cc, j * 128:(j + 1) * 128], wm[:, cc, 128:514], start=(cc == 0), stop=(cc == 15))
                    for cc in range(16):
                        P.mm(ps2[:, 0:256], hb[:, cc, j * 128:(j + 1) * 128], wm[:, cc, 514:770], start=(cc == 0), stop=(cc == 15))
                    qT = qT_all[:, j * 128:(j + 1) * 128]
                    kT = kT_all[:, j * 128:(j + 1) * 128]
                    col = g("col")
                    ktm = g("ktm")
                    P.act(ktm[:, :], ps1[:, 0:128], AF.Copy, scale=128.0 ** -0.5)
                    v1 = g("v1")
                    P.copy(v1[:, 0:256], ps1[:, 128:384])
                    sig = g("sig")
                    P.act(sig[:, :], ps2[:, 0:256], AF.Sigmoid)
                    P.copy(col[:, 12:14], ps1[:, 384:386])
                    P.act(col[:, 0:1], col[:, 12:13], AF.Tanh, bias=mb[:, hd:hd + 1], scale=1.0 / 15.0)
                    P.ts(col[:, 1:2], col[:, 0:1], 15.0, None, ALU.mult)
                    P.ts(col[:, 2:3], col[:, 13:14], mb[:, 8 + hd:9 + hd], None, ALU.add)
                    self.softplus_parts(col[:, 2:3], col[:, 3:4], col[:, 4:5], col[:, 5:6])
                    P.stt(col[:, 6:7], col[:, 2:3], 0.0, col[:, 5:6], ALU.min, ALU.subtract)
                    Fm = g("Fm")
                    fbc = g("fbc")
                    P.ts(Fm[:, :], self.GT, col[:, 6:7], None, ALU.mult)
                    P.ts(fbc[:, :], self.ones, col[:, 6:7], None, ALU.mult)
                    psD = self.q()
                    psE = self.q()
                    P.mm(psD, Fm[:, :], self.U)
                    P.mm(psE, fbc[:, :], self.U)
                    DmT = g("DmT")
                    P.act(DmT[:, :], psD, AF.Exp, bias=col[:, 1:2])
                    DmTm = g("DmTm")
                    P.tt(DmTm[:, :], DmT[:, :], self.U, ALU.mult)
                    eb = g("eb")
                    P.act(eb[:, :], psE, AF.Exp)
                    psS = self.q()
                    P.mm(psS, kT, qT)
                    SmT = g("SmT")
                    P.tt(SmT[:, :], psS, DmTm[:, :], ALU.mult)
                    QdT = g("QdT")
                    P.tt(QdT[:, :], qT, eb[:, :], ALU.mult)
                    psN = self.bank()
                    P.mm(psN[:, 0:257], QdT[:, :], Cn[:, :], start=True, stop=False)
                    P.mm(psN[:, 0:257], SmT[:, :], v1[:, :], start=False, stop=True)
                    kd = g("kd")
                    P.ts(kd[:, :], ktm[:, :], DmT[:, 127:128], None, ALU.mult)
                    psC = self.bank()
                    P.mm(psC[:, 0:257], kd[:, :], v1[:, :])
                    P.stt(Cn[:, :], Cn[:, :], eb[:, 127:128], psC[:, 0:257], ALU.mult, ALU.add)
                    P.copy(col[:, 14:15], psN[:, 256:257])
                    P.stt(col[:, 7:8], col[:, 14:15], -1.0, col[:, 14:15], ALU.mult, ALU.max)
                    P.ts(col[:, 7:8], col[:, 7:8], 1.0, None, ALU.max)
                    P.recip(col[:, 8:9], col[:, 7:8])
                    hraw = g("hraw")
                    P.ts(hraw[:, :], psN[:, 0:256], col[:, 8:9], None, ALU.mult)
                    junk = g("junk")
                    P.act(junk[:, :], hraw[:, :], AF.Square, accum_out=col[:, 9:10])
                    self.rstd_from_ss(col[:, 9:10], 256.0, col[:, 10:11], col[:, 11:12])
                    t1 = g("junk")
                    P.stt(t1[:, :], hraw[:, :], col[:, 11:12], mnw[:, :], ALU.mult, ALU.mult)
                    hm = g("hm")
                    P.tt(hm[:, :], t1[:, :], sig[:, :], ALU.mult)
                    P.dma(self.mix[tok0:tok0 + 128, hd * 256:(hd + 1) * 256], hm[:, :], wr=[P.uniq("mix")])

        xc = P.sbuf("xc", [128, 4, 515], F32)
        cv = P.sbuf("cv", [128, 4, 512], F32)
        cs = P.sbuf("cs", [128, 4, 512], F32)
        sq = P.sbuf("sq", [128, 2, 512], F32)
        rn = P.sbuf("rn", [128, 2, 512], F32)
        Sst = [P.sbuf(f"Sst{i}", [128, 128], F32) for i in range(2)]
        for jh in range(16):
            wg = wbuf[jh % 2]
            P.dma(wg[:, :, :].rearrange("p c n -> p (c n)"), self.w_in_g[jh], eng="pool", max_dma_last_dim=8192)
            P.dma(cw[:, :], self.conv_g[jh])
            P.memset(xc[:, :, 0:3], 0.0)
            for s_ in Sst:
                P.memset(s_[:, :], 0.0)
            hb_next = load_h(0)
            for T in range(NTL):
                hb = hb_next
                if T + 1 < NTL:
                    hb_next = load_h(T + 1)
                for ct in range(4):
                    ps = self.bank()
                    for cc in range(16):
                        P.mm(ps[:, :], wg[:, cc, ct * 128:(ct + 1) * 128], hb[:, cc, :], start=(cc == 0), stop=(cc == 15))
                    P.copy(xc[:, ct, 3:515], ps[:, :], eng="act")
                for ct in range(4):
                    P.ts(cv[:, ct, :], xc[:, ct, 0:512], cw[:, ct * 4:ct * 4 + 1], None, ALU.mult)
                    for k in range(1, 4):
                        P.stt(cv[:, ct, :], xc[:, ct, k:k + 512], cw[:, ct * 4 + k:ct * 4 + k + 1], cv[:, ct, :], ALU.mult, ALU.add)
                P.copy(xc[:, :, 0:3], xc[:, :, 512:515])
                P.act(cs[:, :, :], cv[:, :, :], AF.Silu)
                P.act(sq[:, :, :], cs[:, 0:2, :], AF.Square)
                for ct in range(2):
                    ps = self.bank()
                    P.mm(ps[:, :], self.ones, sq[:, ct, :])
                    P.ts(rn[:, ct, :], ps[:, :], RMS_EPS, None, ALU.add)
                P.act(rn[:, :, :], rn[:, :, :], AF.Sqrt)
                P.recip(rn[:, :, :], rn[:, :, :])
                P.stt(qT_all[:, :], cs[:, 0, :], 128.0 ** -0.5, rn[:, 0, :], ALU.mult, ALU.mult)
                P.tt(kT_all[:, :], cs[:, 1, :], rn[:, 1, :], ALU.mult)
                for j in range(4):
                    tok0 = T * 512 + j * 128
                    sl = slice(j * 128, (j + 1) * 128)
                    ps3 = self.bank()
                    for cc in range(16):
                        P.mm(ps3[:, 0:260], hb[:, cc, sl], wg[:, cc, 512:772], start=(cc == 0), stop=(cc == 15))
                    qT = qT_all[:, sl]
                    kT = kT_all[:, sl]
                    psK = self.q()
                    P.tr(psK, kT, self.ident)
                    ktm = g("ktm")
                    P.copy(ktm[:, :], psK, eng="act")
                    tk3 = g("tk3")
                    P.copy(tk3[:, :], ps3[:, 0:260], eng="act")
                    ps3 = tk3
                    psKK_ = self.q()
                    P.mm(psKK_, kT, kT)
                    psKK = g("KK")
                    P.copy(psKK[:, :], psKK_, eng="act")
                    psKK = psKK[:, :]
                    psAT_ = self.q()
                    P.mm(psAT_, kT, qT)
                    psAT = g("AT")
                    P.copy(psAT[:, :], psAT_)
                    psAT = psAT[:, :]
                    for hv in range(2):
                        hh = 2 * jh + hv
                        col = g("col")
                        P.act(col[:, 0:1], ps3[:, 2 + hv:3 + hv], AF.Sigmoid)
                        P.ts(col[:, 1:2], ps3[:, hv:hv + 1], gab[:, 32 + hh:33 + hh], None, ALU.add)
                        self.softplus_parts(col[:, 1:2], col[:, 2:3], col[:, 3:4], col[:, 4:5])
                        P.stt(col[:, 5:6], col[:, 1:2], 0.0, col[:, 4:5], ALU.max, ALU.add)
                        P.ts(col[:, 6:7], col[:, 5:6], gab[:, hh:hh + 1], None, ALU.mult)
                        Fm = g("Fm")
                        gbc = g("fbc")
                        P.ts(Fm[:, :], self.GT, col[:, 6:7], None, ALU.mult)
                        P.ts(gbc[:, :], self.ones, col[:, 6:7], None, ALU.mult)
                        psD = self.q()
                        psDT = self.q()
                        psEG = self.q()
                        psG = self.q()
                        P.mm(psD, self.U, Fm[:, :])
                        P.mm(psDT, Fm[:, :], self.U)
                        P.mm(psEG, gbc[:, :], self.U)
                        P.mm(psG[:, 0:1], self.U, col[:, 6:7])
                        dec = g("dec")
                        decT = g("decT")
                        eGb = g("eGb")
                        P.act(dec[:, :], psD, AF.Exp)
                        P.act(decT[:, :], psDT, AF.Exp)
                        P.act(eGb[:, :], psEG, AF.Exp)
                        P.act(col[:, 7:8], psG[:, 0:1], AF.Exp)
                        decmb = g("decmb")
                        P.stt(decmb[:, :], dec[:, :], col[:, 0:1], self.GT, ALU.mult, ALU.mult)
                        Nm = g("Nm")
                        P.tt(Nm[:, :], psKK, decmb[:, :], ALU.mult)
                        decTm = g("decTm")
                        P.tt(decTm[:, :], decT[:, :], self.U, ALU.mult)
                        attnT = g("attnT")
                        P.tt(attnT[:, :], psAT, decTm[:, :], ALU.mult)
                        QdT = g("QdT")
                        P.tt(QdT[:, :], qT, eGb[:, :], ALU.mult)
                        psVt = self.q()
                        P.tr(psVt, cs[:, 2 + hv, sl], self.ident)
                        vb = g("vb")
                        P.ts(vb[:, :], psVt, col[:, 0:1], None, ALU.mult)
                        P.tt(col[:, 8:9], col[:, 0:1], col[:, 7:8], ALU.mult)
                        kbg = g("kbg")
                        P.ts(kbg[:, :], ktm[:, :], col[:, 8:9], None, ALU.mult)
                        kdec = g("kdec")
                        P.ts(kdec[:, :], ktm[:, :], decT[:, 127:128], None, ALU.mult)
                        psNT = self.q()
                        P.tr(psNT, Nm[:, :], self.ident)
                        NT_ = g("NT")
                        P.copy(NT_[:, :], psNT, eng="act")
                        tmp = g("tmp")
                        P.tt(tmp[:, :], Nm[:, :], self.negm[0], ALU.mult)
                        Tc = g("T")
                        P.tt(Tc[:, :], tmp[:, :], self.ident, ALU.add)
                        psR = self.q()
                        P.tr(psR, Tc[:, :], self.ident)
                        Rc = g("R")
                        P.copy(Rc[:, :], psR, eng="act")
                        for l in range(1, 7):
                            psY = self.q()
                            P.mm(psY, NT_[:, :], Tc[:, :])
                            Y = g("Y")
                            P.copy(Y[:, :], psY, eng="act")
                            psZ = self.q()
                            P.mm(psZ, Rc[:, :], Y[:, :])
                            tmp = g("tmp")
                            P.tt(tmp[:, :], psZ, self.negm[l], ALU.mult)
                            Tn = g("T")
                            P.tt(Tn[:, :], tmp[:, :], Tc[:, :], ALU.add)
                            Tc = Tn
                            psR = self.q()
                            P.tr(psR, Tc[:, :], self.ident)
                            Rc = g("R")
                            P.copy(Rc[:, :], psR, eng="act")
                        psW = self.q()
                        P.mm(psW, kbg[:, :], Rc[:, :])
                        nwT = g("nwT")
                        P.act(nwT[:, :], psW, AF.Copy, scale=-1.0)
                        Sh = Sst[hv]
                        psV = self.q()
                        P.mm(psV, Rc[:, :], vb[:, :], start=True, stop=False)
                        P.mm(psV, nwT[:, :], Sh[:, :], start=False, stop=True)
                        vnew = g("vnew")
                        P.copy(vnew[:, :], psV, eng="act")
                        psO = self.q()
                        P.mm(psO, QdT[:, :], Sh[:, :], start=True, stop=False)
                        P.mm(psO, attnT[:, :], vnew[:, :], start=False, stop=True)
                        psS = self.q()
                        P.mm(psS, kdec[:, :], vnew[:, :])
                        P.stt(Sh[:, :], Sh[:, :], eGb[:, 127:128], psS, ALU.mult, ALU.add)
                        junk = g("tmp")
                        P.act(junk[:, :], psO, AF.Square, accum_out=col[:, 9:10])
                        self.rstd_from_ss(col[:, 9:10], 128.0, col[:, 10:11], col[:, 11:12])
                        sz = g("sz")
                        P.act(sz[:, :], ps3[:, 4 + hv * 128:4 + (hv + 1) * 128], AF.Silu)
                        t1 = g("t1")
                        P.stt(t1[:, :], psO, col[:, 11:12], gnw[:, :], ALU.mult, ALU.mult)
                        on = g("on")
                        P.tt(on[:, :], t1[:, :], sz[:, :], ALU.mult)
                        P.dma(self.mix[tok0:tok0 + 128, D + hh * 128:D + (hh + 1) * 128], on[:, :], wr=[P.uniq("mix")])
        P.release(mk)

    def phaseB(self):
        P, nc, S, NE = self.P, self.nc, self.S, self.NE
        TB = 256
        mk = P.mark()
        self.set_psum(5, 3)
        hTv = self.hT.rearrange("(c p) s -> p c s", p=128)
        mixtm = P.sbuf("mixtm", [128, 2, 3 * D], BF16)
        mixT = P.sbuf("mixT", [128, 48, TB], BF16)
        hTt = P.sbuf("hTB", [128, 16, TB], BF16)
        xt = P.sbuf("xB", [128, 2, D], F32)
        wa = [P.sbuf(f"waB{i}", [128, 16, 128], BF16) for i in range(2)]
        wb = [P.sbuf(f"wbB{i}", [128, 32, 128], BF16) for i in range(2)]
        wr = [P.sbuf(f"wrB{i}", [128, 16, 256], BF16) for i in range(2)]
        wo = [P.sbuf(f"woB{i}", [128, 16, 512], BF16) for i in range(2)]
        mT = P.sbuf("mergedT", [128, 16, TB], BF16)
        pre = P.sbuf("preB", [128, 2, D], F32)
        g1b = P.sbuf("g1b", [128, D], F32)
        sa = [P.sbuf(f"saB{i}", [128, TB], F32) for i in range(2)]
        sb_ = [P.sbuf(f"sbB{i}", [128, TB], F32) for i in range(2)]
        P.dma(g1b[:, :], self.modrow[0:1, 2 * D:3 * D].partition_broadcast(128))
        wn = 0
        for tb in range(S // TB):
            t0 = tb * TB
            P.dma(mixtm[:, :, :], self.mix[t0:t0 + TB, :].rearrange("(s p) f -> p s f", p=128), rd=[P.uniq("mixr")])
            P.dma(hTt[:, :, :], hTv[:, :, t0:t0 + TB], rd=[P.uniq("hTr")])
            P.dma(xt[:, :, :], self.x[t0:t0 + TB, :].rearrange("(s p) f -> p s f", p=128))
            k = 0
            for s in range(2):
                for f4 in range(12):
                    pb = self.qb()
                    for kk in range(4):
                        fc = f4 * 4 + kk
                        P.tr(pb[:, kk * 128:(kk + 1) * 128], mixtm[:, s, fc * 128:(fc + 1) * 128], self.identb[:, :])
                    P.copy(mixT[:, f4 * 4:f4 * 4 + 4, s * 128:(s + 1) * 128], pb[:, 0:512].rearrange("p (k t) -> p k t", k=4),
                           eng=("act" if k % 2 else "dve"))
                    k += 1
            for n in range(16):
                a_, b_, r_ = wa[wn % 2], wb[wn % 2], wr[wn % 2]
                wn += 1
                P.dma(a_[:, :, :].rearrange("p c n -> p (c n)"), self.w_a[n], eng="pool", max_dma_last_dim=8192)
                P.dma(b_[:, :, :].rearrange("p c n -> p (c n)"), self.w_b[n], eng="pool", max_dma_last_dim=8192)
                P.dma(r_[:, :, :].rearrange("p c n -> p (c n)"), self.w_rarb[n], eng="pool", max_dma_last_dim=8192)
                pA, pB, pRa, pRb = self.bank(), self.bank(), self.bank(), self.bank()
                for cc in range(16):
                    P.mm(pA[:, 0:TB], a_[:, cc, :], mixT[:, cc, :], start=(cc == 0), stop=(cc == 15))
                for cc in range(32):
                    P.mm(pB[:, 0:TB], b_[:, cc, :], mixT[:, 16 + cc, :], start=(cc == 0), stop=(cc == 31))
                for cc in range(16):
                    P.mm(pRa[:, 0:TB], r_[:, cc, 0:128], hTt[:, cc, :], start=(cc == 0), stop=(cc == 15))
                for cc in range(16):
                    P.mm(pRb[:, 0:TB], r_[:, cc, 128:256], hTt[:, cc, :], start=(cc == 0), stop=(cc == 15))
                s1, s2 = sa[n % 2], sb_[n % 2]
                P.act(s1[:, :], pRa[:, 0:TB], AF.Sigmoid)
                P.act(s2[:, :], pRb[:, 0:TB], AF.Sigmoid)
                P.tt(s1[:, :], pA[:, 0:TB], s1[:, :], ALU.mult)
                P.tt(s2[:, :], pB[:, 0:TB], s2[:, :], ALU.mult)
                P.tt(mT[:, n, :], s1[:, :], s2[:, :], ALU.add)
            for cb in range(4):
                o_ = wo[cb % 2]
                P.dma(o_[:, :, :].rearrange("p c n -> p (c n)"), self.w_out[cb], eng="pool", max_dma_last_dim=8192)
                for s in range(2):
                    pM = self.bank()
                    for cc in range(16):
                        P.mm(pM[:, :], mT[:, cc, s * 128:(s + 1) * 128], o_[:, cc, :], start=(cc == 0), stop=(cc == 15))
                    dst = pre[:, s, cb * 512:(cb + 1) * 512]
                    P.tt(dst, pM[:, :], g1b[:, cb * 512:(cb + 1) * 512], ALU.mult)
                    P.stt(dst, xt[:, s, cb * 512:(cb + 1) * 512], ALPHA, dst, ALU.mult, ALU.add)
            P.dma(self.pre[t0:t0 + TB, :].rearrange("(s p) f -> p s f", p=128), pre[:, :, :], wr=[P.uniq("prew")])
        P.release(mk)

        mk = P.mark()
        self.set_psum(8, 0)
        lnb = [P.sbuf(f"lnb{i}", [128, D], F32) for i in range(4)]
        P.dma(lnb[0][:, :], self.lnp[0:1, :].partition_broadcast(128))
        P.dma(lnb[1][:, :], self.lnp[1:2, :].partition_broadcast(128))
        P.dma(lnb[3][:, :], self.modrow[0:1, 3 * D:4 * D].partition_broadcast(128))
        P.dma(lnb[2][:, :], self.modrow[0:1, 4 * D:5 * D].partition_broadcast(128))
        P.ts(lnb[2][:, :], lnb[2][:, :], 1.0, None, ALU.add)
        wrt = P.sbuf("wrt", [128, 16, NE], F32)
        P.dma(wrt[:, :, :], self.w_router.rearrange("(c p) n -> p c n", p=128))
        brb = P.sbuf("brb", [128, NE], F32)
        P.dma(brb[:, :], self.b_router[0:1, :].partition_broadcast(128))
        pt_ = [P.sbuf(f"pre2_{i}", [128, D], F32) for i in range(2)]
        x1t = [P.sbuf(f"x1t_{i}", [128, D], F32) for i in range(2)]
        h2t = [P.sbuf(f"h2t_{i}", [128, D], F32) for i in range(2)]
        h2Tf = [P.sbuf(f"h2Tf_{i}", [128, 16, 128], F32) for i in range(2)]
        h2Tb = [P.sbuf(f"h2Tb_{i}", [128, 16, 128], BF16) for i in range(2)]
        st = P.sbuf("stB", [128, 24], F32)
        mv = P.sbuf("mvB", [128, 8], F32)
        lg = [P.sbuf(f"lg{i}", [128, 3, NE], F32) for i in range(2)]
        m8 = P.sbuf("m8", [128, 16], F32)
        h2Tv = self.h2T.rearrange("(c p) s -> p c s", p=128)
        for t in range(self.NT):
            t0 = t * 128
            p_, x_, h_ = pt_[t % 2], x1t[t % 2], h2t[t % 2]
            P.dma(p_[:, :], self.pre[t0:t0 + 128, :], rd=[P.uniq("prer")])
            self.ln_stats(p_, st, mv, mv[:, 4:5], mv[:, 5:6], LN_EPS)
            P.act(x_[:, :], p_[:, :], AF.Identity, bias=mv[:, 5:6], scale=mv[:, 4:5])
            P.tt(x_[:, :], x_[:, :], lnb[0][:, :], ALU.mult)
            P.tt(x_[:, :], x_[:, :], lnb[1][:, :], ALU.add)
            P.dma(self.x1[t0:t0 + 128, :], x_[:, :], wr=[P.uniq("x1w")])
            self.ln_stats(x_, st, mv, mv[:, 4:5], mv[:, 5:6], LN_EPS)
            P.act(h_[:, :], x_[:, :], AF.Identity, bias=mv[:, 5:6], scale=mv[:, 4:5])
            P.tt(h_[:, :], h_[:, :], lnb[2][:, :], ALU.mult)
            P.tt(h_[:, :], h_[:, :], lnb[3][:, :], ALU.add)
            hf, hb = h2Tf[t % 2], h2Tb[t % 2]
            for cc in range(16):
                pq = self.q()
                P.tr(pq, h_[:, cc * 128:(cc + 1) * 128], self.ident)
                P.copy(hf[:, cc, :], pq, eng="act")
                P.copy(hb[:, cc, :], pq, eng="dve")
            P.dma(h2Tv[:, :, t0:t0 + 128], hb[:, :, :], wr=[P.uniq("h2Tw")])
            pl = self.q()
            for cc in range(16):
                P.mm(pl[:, 0:NE], hf[:, cc, :], wrt[:, cc, :], start=(cc == 0), stop=(cc == 15))
            l_ = lg[t % 2]
            P.tt(l_[:, 0, :], pl[:, 0:NE], brb[:, :], ALU.add)
            P.add("dve", (lambda l_=l_: nc.vector.max(m8[:, 0:8], l_[:, 0, :])), reads=[l_[:, 0, :]], writes=[m8[:, 0:8]])
            P.ts(l_[:, 1, :], l_[:, 0, :], m8[:, 3:4], None, ALU.is_ge)
            P.ts(m8[:, 8:9], m8[:, 0:1], -1.0, None, ALU.mult)
            P.act(l_[:, 2, :], l_[:, 0, :], AF.Exp, bias=m8[:, 8:9])
            P.tt(l_[:, 2, :], l_[:, 2, :], l_[:, 1, :], ALU.mult)
            P.add("dve", (lambda l_=l_: nc.vector.reduce_sum(m8[:, 9:10], l_[:, 2, :], mybir.AxisListType.X)),
                  reads=[l_[:, 2, :]], writes=[m8[:, 9:10]])
            P.recip(m8[:, 10:11], m8[:, 9:10])
            P.ts(l_[:, 0, :], l_[:, 2, :], m8[:, 10:11], None, ALU.mult)
            P.dma(self.gates_d[t0:t0 + 128, :], l_[:, 0, :], wr=[P.uniq("gw")])
        P.release(mk)

    def phaseC(self):
        P, nc, S, NE = self.P, self.nc, self.S, self.NE
        TG = min(1024, S)
        NS = TG // 128
        NH = TG // 512
        mk = P.mark()
        self.set_psum(8, 0)
        h2Tv = self.h2T.rearrange("(c p) s -> p c s", p=128)
        acc = P.sbuf("acc", [128, NS, D], F32)
        h2g = P.sbuf("h2g", [128, 16, TG], BF16)
        actT = P.sbuf("actT", [128, 16, TG], BF16)
        wu = [P.sbuf(f"wu{i}", [128, 16, 256], BF16) for i in range(2)]
        wd = [P.sbuf(f"wd{i}", [128, 16, 256], BF16) for i in range(2)]
        gt = P.sbuf("gt", [128, NS, NE], F32)
        gTs = P.sbuf("gTs", [NE, 128], F32)
        bd = P.sbuf("bd", [NE, D], F32)
        bg = P.sbuf("bg", [128, NE * 16], F32)
        bu = P.sbuf("bu", [128, NE * 16], F32)
        P.dma(bd[:, :], self.b_down[:, :])
        P.dma(bg[:, :], self.b_upg[:, :])
        P.dma(bu[:, :], self.b_upu[:, :])
        ew = [[P.sbuf(f"ew{k}_{i}", [128, 512], F32) for i in range(2)] for k in range(3)]
        wun = 0
        wdn = 0
        en = 0
        for G in range(S // TG):
            g0 = G * TG
            P.dma(h2g[:, :, :], h2Tv[:, :, g0:g0 + TG], rd=[P.uniq("h2Tr")])
            P.dma(gt[:, :, :], self.gates_d[g0:g0 + TG, :].rearrange("(s p) e -> p s e", p=128), rd=[P.uniq("gr")])
            for s in range(NS):
                pq = self.q()
                P.tr(pq[0:NE, :], gt[:, s, :], self.ident)
                P.copy(gTs[:, :], pq[0:NE, :], eng="act")
                for cb in range(4):
                    pb = self.bank()
                    P.mm(pb[:, :], gTs[:, :], bd[:, cb * 512:(cb + 1) * 512])
                    P.copy(acc[:, s, cb * 512:(cb + 1) * 512], pb[:, :], eng=("act" if cb % 2 else "dve"))
            for e in range(NE):
                for jt in range(16):
                    w_ = wu[wun % 2]
                    wun += 1
                    P.dma(w_[:, :, :].rearrange("p c n -> p (c n)"), self.w_ups[e // self.EG][e % self.EG, jt], eng="pool", max_dma_last_dim=8192)
                    for hf in range(NH):
                        tsl = slice(hf * 512, (hf + 1) * 512)
                        pG, pU = self.bank(), self.bank()
                        for cc in range(16):
                            P.mm(pG[:, :], w_[:, cc, 0:128], h2g[:, cc, tsl], start=(cc == 0), stop=(cc == 15))
                        for cc in range(16):
                            P.mm(pU[:, :], w_[:, cc, 128:256], h2g[:, cc, tsl], start=(cc == 0), stop=(cc == 15))
                        g_, s_, u_ = ew[0][en % 2], ew[1][en % 2], ew[2][en % 2]
                        en += 1
                        bi = e * 16 + jt
                        P.ts(g_[:, :], pG[:, :], bg[:, bi:bi + 1], 7.0, ALU.add, ALU.min)
                        P.act(s_[:, :], g_[:, :], AF.Sigmoid, scale=1.702)
                        P.ts(u_[:, :], pU[:, :], bu[:, bi:bi + 1], 7.0, ALU.add, ALU.min)
                        P.ts(u_[:, :], u_[:, :], -7.0, 1.0, ALU.max, ALU.add)
                        P.tt(g_[:, :], g_[:, :], s_[:, :], ALU.mult)
                        P.tt(actT[:, jt, tsl], g_[:, :], u_[:, :], ALU.mult)
                for cb in range(8):
                    d_ = wd[wdn % 2]
                    wdn += 1
                    P.dma(d_[:, :, :].rearrange("p c n -> p (c n)"), self.w_downs[e // self.EG][e % self.EG, cb], eng="pool", max_dma_last_dim=8192)
                    for s in range(NS):
                        pb = self.bank()
                        for cc in range(16):
                            P.mm(pb[:, 0:256], actT[:, cc, s * 128:(s + 1) * 128], d_[:, cc, :], start=(cc == 0), stop=(cc == 15))
                        dst = acc[:, s, cb * 256:(cb + 1) * 256]
                        P.stt(dst, pb[:, 0:256], gt[:, s, e:e + 1], dst, ALU.mult, ALU.add)
            P.dma(self.ffn[g0:g0 + TG, :].rearrange("(s p) f -> p s f", p=128), acc[:, :, :], wr=[P.uniq("ffnw")])
        P.release(mk)
        mk = P.mark()
        fb = [P.sbuf(f"fb{i}", [128, D], F32) for i in range(3)]
        P.dma(fb[0][:, :], self.modrow[0:1, 5 * D:6 * D].partition_broadcast(128))
        P.dma(fb[1][:, :], self.lnp[2:3, :].partition_broadcast(128))
        P.dma(fb[2][:, :], self.lnp[3:4, :].partition_broadcast(128))
        st = P.sbuf("stD", [128, 24], F32)
        mv = P.sbuf("mvD", [128, 8], F32)
        fa = [P.sbuf(f"fa{i}", [128, D], F32) for i in range(2)]
        x1b = [P.sbuf(f"x1b{i}", [128, D], F32) for i in range(2)]
        for t in range(self.NT):
            t0 = t * 128
            a_, x_ = fa[t % 2], x1b[t % 2]
            P.dma(a_[:, :], self.ffn[t0:t0 + 128, :], rd=[P.uniq("ffnr")])
            P.dma(x_[:, :], self.x1[t0:t0 + 128, :], rd=[P.uniq("x1r")])
            P.tt(a_[:, :], a_[:, :], fb[0][:, :], ALU.mult)
            P.stt(a_[:, :], x_[:, :], ALPHA, a_[:, :], ALU.mult, ALU.add)
            self.ln_stats(a_, st, mv, mv[:, 4:5], mv[:, 5:6], LN_EPS)
            P.act(x_[:, :], a_[:, :], AF.Identity, bias=mv[:, 5:6], scale=mv[:, 4:5])
            P.tt(x_[:, :], x_[:, :], fb[1][:, :], ALU.mult)
            P.tt(x_[:, :], x_[:, :], fb[2][:, :], ALU.add)
            P.dma(self.out[t0:t0 + 128, :], x_[:, :])
        P.release(mk)


def _prep_inputs(inputs, b, S, NE, shared=None):
    f = lambda a: np.ascontiguousarray(a, dtype=np.float32)
    if shared is not None:
        d = dict(shared)
        d["x"] = f(inputs["x"][b][:S])
        d["c"] = f(inputs["c"][b].reshape(128, 16))
        return d
    w_in = inputs["w_in"][0]
    wm = []
    for hd in range(8):
        cols = np.r_[hd * 128:(hd + 1) * 128, 1024 + hd * 128:1024 + (hd + 1) * 128,
                     2048 + hd * 256:2048 + (hd + 1) * 256, 4096 + hd, 4104 + hd,
                     4112 + hd * 256:4112 + (hd + 1) * 256]
        wm.append(w_in[:, cols])
    wg = []
    G0 = 6160
    for j in range(16):
        cols = np.r_[G0 + j * 128:G0 + (j + 1) * 128, G0 + 2048 + j * 128:G0 + 2048 + (j + 1) * 128,
                     G0 + 4096 + 2 * j * 128:G0 + 4096 + (2 * j + 2) * 128,
                     14352 + 2 * j:14352 + 2 * j + 2, 14384 + 2 * j:14384 + 2 * j + 2,
                     14416 + 2 * j * 128:14416 + (2 * j + 2) * 128]
        wg.append(w_in[:, cols])
    wrr = [np.concatenate([w_in[:, 18512 + n * 128:18512 + (n + 1) * 128],
                           w_in[:, 20560 + n * 128:20560 + (n + 1) * 128]], axis=1) for n in range(16)]
    conv = inputs["conv_w"][0]
    cg = []
    for j in range(16):
        tiles = [conv[:, j * 128:(j + 1) * 128], conv[:, 2048 + j * 128:2048 + (j + 1) * 128],
                 conv[:, 4096 + 2 * j * 128:4096 + (2 * j + 1) * 128], conv[:, 4096 + (2 * j + 1) * 128:4096 + (2 * j + 2) * 128]]
        cg.append(np.concatenate([t.T for t in tiles], axis=1))
    w_up = inputs["w_up"][0][:NE]
    w_up_r = w_up.reshape(NE, 16, 128, 16, 128, 2).transpose(0, 3, 2, 1, 5, 4).reshape(NE, 16, 128, 4096)
    w_down_r = inputs["w_down"][0][:NE].reshape(NE, 16, 128, 8, 256).transpose(0, 3, 2, 1, 4).reshape(NE, 8, 128, 4096)

    def pc(a, ncol):
        kc = a.shape[0] // 128
        return a.reshape(kc, 128, ncol).transpose(1, 0, 2).reshape(128, kc * ncol)
    b_up = inputs["b_up"][0][:NE].reshape(NE, 16, 128, 2)
    EG = min(8, NE)
    extra = {}
    for i in range(NE // EG):
        extra[f"w_up{i}"] = f(w_up_r[i * EG:(i + 1) * EG])
        extra[f"w_down{i}"] = f(w_down_r[i * EG:(i + 1) * EG])
    return {
        **extra,
        "x": f(inputs["x"][b][:S]),
        "c": f(inputs["c"][b].reshape(128, 16)),
        "w_ada": f(inputs["w_ada"][0]),
        "b_ada": f(inputs["b_ada"][0][None, :]),
        "w_in_m": f(np.stack([pc(a, 770) for a in wm])),
        "w_in_g": f(np.stack([pc(a, 772) for a in wg])),
        "w_rarb": f(np.stack([pc(a, 256) for a in wrr])),
        "m_bias": f(np.concatenate([inputs["m_bias_i"][0], inputs["m_bias_f"][0]])[None, :]),
        "m_norm_w": f(inputs["m_norm_w"][0][None, :]),
        "conv_g": f(np.stack(cg)),
        "g_ab": f(np.concatenate([inputs["g_a_log"][0], inputs["g_dt_bias"][0]])[None, :]),
        "g_norm_w": f(inputs["g_norm_w"][0][None, :]),
        "w_a": f(np.stack([pc(inputs["w_branch_a"][0][:, n * 128:(n + 1) * 128], 128) for n in range(16)])),
        "w_b": f(np.stack([pc(inputs["w_branch_b"][0][:, n * 128:(n + 1) * 128], 128) for n in range(16)])),
        "w_out": f(np.stack([pc(inputs["w_out"][0][:, cb * 512:(cb + 1) * 512], 512) for cb in range(4)])),
        "lnp": f(np.stack([inputs["ln1_g"][0], inputs["ln1_b"][0], inputs["ln2_g"][0], inputs["ln2_b"][0]])),
        "w_router": f(inputs["w_router"][0][:, :NE]),
        "b_router": f(inputs["b_router"][0][None, :NE]),
        "b_upg": f(b_up[:, :, :, 0].transpose(2, 0, 1).reshape(128, NE * 16)),
        "b_upu": f(b_up[:, :, :, 1].transpose(2, 0, 1).reshape(128, NE * 16)),
        "b_down": f(inputs["b_down"][0][:NE]),
        "consts": make_consts(),
    }


def run(inputs, S=4096, NE=32, cores=(0, 1), debug=False, phases="0ABC", trace=False):
    bld = Builder(S, NE, debug=debug, phases=phases)
    first = _prep_inputs(inputs, cores[0], S, NE)
    in_maps = [{k: v for k, v in _prep_inputs(inputs, b, S, NE, shared=first).items() if k in bld.decl} for b in cores]
    res = run_bass_kernel_spmd(bld.nc, in_maps, core_ids=list(range(len(cores))), trace=trace)
    return res, bld


def kernel(**inputs):
    inputs = {k: np.asarray(v) for k, v in inputs.items()}
    res, _ = run(inputs)
    return np.stack([np.asarray(r["out"], dtype=np.float32) for r in res.results], axis=0)
```

```python
import numpy as np
import concourse.bass as bass
import concourse.mybir as mybir
from concourse.bass_utils import run_bass_kernel_spmd

F32 = mybir.dt.float32
BF16 = mybir.dt.bfloat16
AF = mybir.ActivationFunctionType
ALU = mybir.AluOpType

SEM_PHASE = 12000
DMA_POOL = 24

D = 2048
DC = 16
IN_W = 22608
LN_EPS = 1e-5
RMS_EPS = 1e-6
ALPHA = 2.0 ** 0.25
NCONST = 11 * 128


class Ins:
    __slots__ = ("eng", "fn", "reads", "writes", "dma", "idx", "deps", "sig", "semv", "name")


class Prog:
    def __init__(self, nc):
        self.nc = nc
        self.ins = []
        self.tinfo = {}
        self.engs = {"pe": nc.tensor, "act": nc.scalar, "dve": nc.vector, "pool": nc.gpsimd, "sp": nc.sync}
        self._ctx = []
        self.self_sync = {"dve", "act", "pool"}
        self.uid = 0

    def sbuf(self, name, shape, dtype):
        g = self.nc.sbuf_tensor(name, list(shape), dtype)
        t = g.__enter__()
        self._ctx.append(g)
        self.tinfo[t.name] = (int(np.prod(shape[1:])) * mybir.dt.size(dtype), "sb")
        return t

    def psum(self, name, shape, dtype):
        g = self.nc.psum_tensor(name, list(shape), dtype)
        t = g.__enter__()
        self._ctx.append(g)
        self.tinfo[t.name] = (int(np.prod(shape[1:])) * mybir.dt.size(dtype), "ps")
        return t

    def dram(self, name, shape, dtype, kind="Internal"):
        t = self.nc.dram_tensor(name, list(shape), dtype, kind=kind)
        self.tinfo[t.name] = (None, "dr")
        return t

    def mark(self):
        return len(self._ctx)

    def release(self, mark):
        self.barrier()
        while len(self._ctx) > mark:
            g = self._ctx.pop()
            g.__exit__(None, None, None)

    def uniq(self, tag):
        self.uid += 1
        return (tag, 0, 1, self.uid, self.uid + 1)

    def region(self, ap):
        name = ap.tensor.name
        info = self.tinfo.get(name)
        if info is None:
            return None
        fe, sp = info
        pat = ap.ap
        es = mybir.dt.size(ap.dtype)
        off = int(ap.offset) * es
        if sp == "dr":
            ext = es
            for st, cnt in pat:
                ext += (cnt - 1) * abs(st) * es
            return (name, 0, 1, off, off + ext)
        if sp == "ps":
            return (name, 0, 128, 0, fe)
        pst, pcnt = pat[0]
        pst *= es
        p0 = off // fe
        f0 = off % fe
        pstep = max(1, pst // fe) if pst else 0
        p1 = p0 + (pcnt - 1) * pstep + 1
        ext = es
        for st, cnt in pat[1:]:
            ext += (cnt - 1) * abs(st) * es
        return (name, p0, p1, f0, f0 + ext)

    def add(self, eng, fn, reads=(), writes=(), dma=False, name=""):
        i = Ins()
        i.eng = eng
        i.fn = fn
        i.reads = [r for r in (a if isinstance(a, tuple) else self.region(a) for a in reads) if r is not None]
        i.writes = [r for r in (a if isinstance(a, tuple) else self.region(a) for a in writes) if r is not None]
        i.dma = dma
        i.idx = len(self.ins)
        i.deps = set()
        i.sig = False
        i.semv = None
        i.name = name
        self.ins.append(i)
        return i

    def barrier(self):
        self.add("sp", lambda: self.nc.sync.nop(), writes=[("BAR", 0, 1, 0, 1)], name="bar1")
        for e in ("pe", "act", "dve", "pool"):
            self.add(e, lambda: None, reads=[("BAR", 0, 1, 0, 1)], name="bar2")

    def mm(self, out, lhsT, rhs, start=True, stop=True):
        return self.add("pe", lambda: self.nc.tensor.matmul(out, lhsT, rhs, start=start, stop=stop),
                        reads=[lhsT, rhs] + ([] if start else [out]), writes=[out])

    def tr(self, out, in_, ident):
        return self.add("pe", lambda: self.nc.tensor.transpose(out, in_, ident), reads=[in_, ident], writes=[out])

    def act(self, out, in_, func, bias=None, scale=None, accum_out=None):
        kw = {}
        rd = [in_]
        if bias is not None:
            kw["bias"] = bias
            if not isinstance(bias, (int, float)):
                rd.append(bias)
        if scale is not None:
            kw["scale"] = scale
            if not isinstance(scale, (int, float)):
                rd.append(scale)
        wr = [out]
        if accum_out is not None:
            kw["accum_out"] = accum_out
            wr.append(accum_out)
        return self.add("act", lambda: self.nc.scalar.activation(out, in_, func, **kw), reads=rd, writes=wr)

    def _veng(self, eng):
        return self.nc.vector if eng == "dve" else self.nc.gpsimd

    def ts(self, out, in0, s1, s2, op0, op1=None, eng="dve", accum_out=None):
        rd = [in0] + [s for s in (s1, s2) if s is not None and not isinstance(s, (int, float))]
        kw = {}
        wr = [out]
        if accum_out is not None:
            kw["accum_out"] = accum_out
            wr.append(accum_out)
        if op1 is None:
            return self.add(eng, lambda: self._veng(eng).tensor_scalar(out, in0, s1, None, op0, **kw), reads=rd, writes=wr)
        return self.add(eng, lambda: self._veng(eng).tensor_scalar(out, in0, s1, s2, op0, op1, **kw), reads=rd, writes=wr)

    def tt(self, out, in0, in1, op, eng="dve"):
        return self.add(eng, lambda: self._veng(eng).tensor_tensor(out, in0, in1, op), reads=[in0, in1], writes=[out])

    def stt(self, out, in0, scalar, in1, op0, op1):
        rd = [in0, in1] + ([] if isinstance(scalar, (int, float)) else [scalar])
        return self.add("dve", lambda: self.nc.vector.scalar_tensor_tensor(out, in0, scalar, in1, op0, op1), reads=rd, writes=[out])

    def copy(self, out, in_, eng="dve"):
        if eng == "act":
            return self.add("act", lambda: self.nc.scalar.copy(out, in_), reads=[in_], writes=[out])
        return self.add(eng, lambda: self._veng(eng).tensor_copy(out, in_), reads=[in_], writes=[out])

    def memset(self, out, val, eng="dve"):
        return self.add(eng, lambda: self._veng(eng).memset(out, val), reads=[], writes=[out])

    def recip(self, out, in_):
        return self.add("dve", lambda: self.nc.vector.reciprocal(out, in_), reads=[in_], writes=[out])

    def dma(self, out, in_, eng="sp", rd=None, wr=None, **kw):
        return self.add(eng, lambda: self.engs[eng].dma_start(out, in_, **kw),
                        reads=[in_] if rd is None else rd, writes=[out] if wr is None else wr, dma=True)

    def emit(self):
        nc = self.nc
        recs = {}
        last_on = {}
        dmas_open = []

        def ovl(r, q):
            return r[0] < q[2] and q[1] < r[1] and r[2] < q[4] and q[3] < r[3]

        for ins in self.ins:
            deps = ins.deps
            if ins.name == "bar1":
                deps.update(last_on.values())
                deps.update(dmas_open)
                dmas_open = []
                recs = {}
            for q in ins.reads:
                isps = self.tinfo.get(q[0], (0, ""))[1] == "ps"
                for r in recs.get(q[0], ()):
                    if ovl(r, q):
                        if r[4] is not None:
                            deps.add(r[4])
                        if isps:
                            deps.update(j for e_, j in r[5].items() if e_ != ins.eng)
                        if ins.dma:
                            r[6].append(ins.idx)
                        else:
                            r[5][ins.eng] = ins.idx
            for q in ins.writes:
                lst = recs.get(q[0], ())
                keep = []
                for r in lst:
                    if ovl(r, q):
                        if r[4] is not None:
                            deps.add(r[4])
                        deps.update(r[5].values())
                        deps.update(r[6])
                        if q[1] <= r[0] and r[1] <= q[2] and q[3] <= r[2] and r[3] <= q[4]:
                            continue
                    keep.append(r)
                keep.append([q[1], q[2], q[3], q[4], ins.idx, {}, []])
                recs[q[0]] = keep
            deps.discard(ins.idx)
            if ins.dma:
                dmas_open.append(ins.idx)
            elif ins.name not in ("bar1", "bar2"):
                last_on[ins.eng] = ins.idx

        for ins in self.ins:
            nd = set()
            for j in ins.deps:
                p = self.ins[j]
                if p.eng == ins.eng and not p.dma and (ins.eng not in self.self_sync):
                    continue
                nd.add(j)
            ins.deps = nd
            for j in nd:
                self.ins[j].sig = True

        self._sems = []

        def newsem(nm):
            g = nc.semaphore(nm)
            s = g.__enter__()
            self._sems.append(g)
            return s

        eng_sems = {e: [] for e in self.engs}
        eng_cnt = {e: 0 for e in self.engs}
        dma_sems = [newsem(f"dq{k}") for k in range(DMA_POOL)]
        dma_n = 0
        known = {e: {} for e in self.engs}

        def wait(engname, sem, val):
            k = known[engname]
            key = id(sem)
            if k.get(key, 0) >= val:
                return
            k[key] = val
            self.engs[engname].wait_ge(sem, val)

        for ins in self.ins:
            e = ins.eng
            for j in sorted(ins.deps):
                p = self.ins[j]
                if p.semv is None:
                    raise RuntimeError(f"dep on unsignaled instr {p.name} {p.eng}")
                wait(e, p.semv[0], p.semv[1])
            if ins.dma:
                slot = dma_n % DMA_POOL
                val = 16 * (dma_n // DMA_POOL + 1)
                if val > 16:
                    wait(e, dma_sems[slot], val - 16)
                dma_n += 1
                ins.semv = (dma_sems[slot], val)
                ins.fn().then_inc(dma_sems[slot], 16)
            else:
                bi = ins.fn()
                if ins.sig:
                    if bi is None:
                        raise RuntimeError("signal needed on empty instr")
                    c = eng_cnt[e]
                    ph = c // SEM_PHASE
                    while len(eng_sems[e]) <= ph:
                        eng_sems[e].append(newsem(f"s_{e}{len(eng_sems[e])}"))
                    sem = eng_sems[e][ph]
                    eng_cnt[e] = c + 1
                    ins.semv = (sem, c % SEM_PHASE + 1)
                    bi.then_inc(sem, 1)
        self.n_dma = dma_n
        self.counts = dict(eng_cnt)


def make_consts():
    i = np.arange(128)
    ident = np.eye(128, dtype=np.float32)
    U = (i[:, None] <= i[None, :]).astype(np.float32)
    GT = (i[:, None] > i[None, :]).astype(np.float32)
    lv = []
    for l in range(7):
        b = 1 << l
        m = ((i[:, None] // (2 * b) == i[None, :] // (2 * b)) & ((i[:, None] // b) % 2 == 1)
             & ((i[None, :] // b) % 2 == 0))
        lv.append(-m.astype(np.float32))
    ones = np.ones((128, 128), np.float32)
    return np.concatenate([ident, U, GT] + lv + [ones], axis=1)


class Builder:
    def __init__(self, S, NE, debug=False, phases="0ABC"):
        self.S = S
        self.NE = NE
        self.NT = S // 128
        self.debug = debug
        self.phases = phases
        nc = bass.Bass("TRN2", target_bir_lowering=False)
        self.nc = nc
        self.P = Prog(nc)
        self.decl = set()
        self.build()

    def din(self, name, shape, dtype=F32, ph="0ABC"):
        if not any(p in self.phases for p in ph):
            return None
        self.decl.add(name)
        return self.nc.dram_tensor(name, list(shape), dtype, kind="ExternalInput").ap()

    def scratch(self, name, shape, dtype):
        kind = "ExternalOutput" if self.debug else "Internal"
        t = self.nc.dram_tensor(name, list(shape), dtype, kind=kind)
        self.P.tinfo[t.name] = (None, "dr")
        return t.ap()

    def build(self):
        P, nc, S, NE = self.P, self.nc, self.S, self.NE
        self.x = self.din("x", [S, D])
        self.c_in = self.din("c", [128, 16])
        self.w_ada = self.din("w_ada", [D, 6 * D], ph="0")
        self.b_ada = self.din("b_ada", [1, 6 * D], ph="0")
        self.w_in_m = self.din("w_in_m", [8, 128, 16 * 770], ph="A")
        self.w_in_g = self.din("w_in_g", [16, 128, 16 * 772], ph="A")
        self.w_rarb = self.din("w_rarb", [16, 128, 4096], ph="B")
        self.m_bias = self.din("m_bias", [1, 16], ph="A")
        self.m_norm_w = self.din("m_norm_w", [1, D], ph="A")
        self.conv_g = self.din("conv_g", [16, 128, 16], ph="A")
        self.g_ab = self.din("g_ab", [1, 64], ph="A")
        self.g_norm_w = self.din("g_norm_w", [1, 128], ph="A")
        self.w_a = self.din("w_a", [16, 128, 2048], ph="B")
        self.w_b = self.din("w_b", [16, 128, 4096], ph="B")
        self.w_out = self.din("w_out", [4, 128, 8192], ph="B")
        self.lnp = self.din("lnp", [4, D])
        self.w_router = self.din("w_router", [D, NE], ph="B")
        self.b_router = self.din("b_router", [1, NE], ph="B")
        self.EG = min(8, NE)
        self.w_ups = [self.din(f"w_up{i}", [self.EG, 16, 128, 4096], ph="C") for i in range(NE // self.EG)]
        self.b_upg = self.din("b_upg", [128, NE * 16], ph="C")
        self.b_upu = self.din("b_upu", [128, NE * 16], ph="C")
        self.w_downs = [self.din(f"w_down{i}", [self.EG, 8, 128, 4096], ph="C") for i in range(NE // self.EG)]
        self.b_down = self.din("b_down", [NE, D], ph="C")
        self.consts_in = self.din("consts", [128, NCONST])
        self.out = self.nc.dram_tensor("out", [S, D], F32, kind="ExternalOutput").ap()
        P.tinfo[self.out.tensor.name] = (None, "dr")

        self.modrow = self.scratch("modrow", [1, 6 * D], F32)
        self.hT = self.scratch("hT", [D, S], BF16)
        self.mix = self.scratch("mix", [S, 3 * D], BF16)
        self.pre = self.scratch("pre", [S, D], F32)
        self.x1 = self.scratch("x1", [S, D], F32)
        self.h2T = self.scratch("h2T", [D, S], BF16)
        self.gates_d = self.scratch("gates", [S, NE], F32)
        self.ffn = self.scratch("ffn", [S, D], F32)

        self.cst = P.sbuf("cst", [128, NCONST], F32)
        P.dma(self.cst[:, :], self.consts_in[:, :])
        c = self.cst
        self.ident = c[:, 0:128]
        self.U = c[:, 128:256]
        self.GT = c[:, 256:384]
        self.negm = [c[:, 384 + 128 * l: 512 + 128 * l] for l in range(7)]
        self.ones = c[:, 1280:1408]
        self.identb = P.sbuf("identb", [128, 128], BF16)
        P.copy(self.identb[:, :], self.ident)
        self.bi = 0
        self.qbi = 0
        self.pn = 0

        if "0" in self.phases:
            self.phase0()
        if "A" in self.phases:
            self.phaseA()
        if "B" in self.phases:
            self.phaseB()
        if "C" in self.phases:
            self.phaseC()
        P.add("sp", lambda: None, reads=[self.out[:, :]] if "C" in self.phases else [], writes=[], name="fin")
        P.barrier()
        P.emit()

    def set_psum(self, nf, nb):
        self.pn += 1
        self.big = [self.P.psum(f"pf{self.pn}_{i}", [128, 512], F32) for i in range(nf)]
        self.bfb = [self.P.psum(f"pb{self.pn}_{i}", [128, 1024], BF16) for i in range(nb)]

    def bank(self):
        b = self.big[self.bi % len(self.big)]
        self.bi += 1
        return b

    def q(self, n=128):
        return self.bank()[:, 0:n]

    def ln_stats(self, src, st, mv, rstd, nmr, eps):
        P, nc = self.P, self.nc
        for k in range(4):
            P.add("dve", (lambda k=k: nc.vector.bn_stats(st[:, k * 6:(k + 1) * 6], src[:, k * 512:(k + 1) * 512])),
                  reads=[src[:, k * 512:(k + 1) * 512]], writes=[st[:, k * 6:(k + 1) * 6]])
        P.add("dve", lambda: nc.vector.bn_aggr(mv[:, 0:2], st[:, 0:24]), reads=[st[:, 0:24]], writes=[mv[:, 0:2]])
        P.ts(mv[:, 2:3], mv[:, 1:2], eps, None, ALU.add)
        P.act(mv[:, 3:4], mv[:, 2:3], AF.Sqrt)
        P.recip(rstd, mv[:, 3:4])
        P.ts(nmr, mv[:, 0:1], -1.0, rstd, ALU.mult, ALU.mult)

    def rstd_from_ss(self, ss, n, tmp, rstd):
        P = self.P
        P.ts(tmp, ss, 1.0 / n, RMS_EPS, ALU.mult, ALU.add)
        P.act(tmp, tmp, AF.Sqrt)
        P.recip(rstd, tmp)

    def softplus_parts(self, y, a, e, l):
        P = self.P
        P.stt(a, y, -1.0, y, ALU.mult, ALU.max)
        P.act(e, a, AF.Exp, scale=-1.0)
        P.ts(e, e, 1.0, None, ALU.add)
        P.act(l, e, AF.Ln)

    def phase0(self):
        import os
        BIS = int(os.environ.get("BIS", "255"))
        P, nc, S = self.P, self.nc, self.S
        mk = P.mark()
        self.set_psum(2, 4)
        csb = P.sbuf("csb", [128, 16], F32)
        scs = P.sbuf("scs", [128, 16], F32)
        P.dma(csb[:, :], self.c_in[:, :])
        P.act(scs[:, :], csb[:, :], AF.Silu)
        wt = [P.sbuf(f"wada{i}", [128, 16, 512], F32) for i in range(2)]
        bt = [P.sbuf(f"bada{i}", [1, 512], F32) for i in range(2)]
        row = [P.sbuf(f"mrow{i}", [1, 512], F32) for i in range(2)]
        wv = self.w_ada.rearrange("(p c) n -> p c n", c=16)
        for n in range(24 if BIS & 1 else 0):
            w = wt[n % 2]
            P.dma(w[:, :, :], wv[:, :, n * 512:(n + 1) * 512])
            ps = self.bank()
            for cc in range(16):
                P.mm(ps[0:1, :], scs[:, cc:cc + 1], w[:, cc, :], start=(cc == 0), stop=(cc == 15))
            P.dma(bt[n % 2][:, :], self.b_ada[0:1, n * 512:(n + 1) * 512])
            P.tt(row[n % 2][0:1, :], ps[0:1, :], bt[n % 2][0:1, :], ALU.add)
            P.dma(self.modrow[0:1, n * 512:(n + 1) * 512], row[n % 2][:, :])
        sc1 = P.sbuf("sc1T", [128, 16], F32)
        sh1 = P.sbuf("sh1T", [128, 16], F32)
        if BIS & 2:
            P.dma(sh1[:, :], self.modrow[0, 0:D].rearrange("(c p) -> p c", p=128), allow_slow_non_contiguous=True)
            P.dma(sc1[:, :], self.modrow[0, D:2 * D].rearrange("(c p) -> p c", p=128), allow_slow_non_contiguous=True)
        else:
            P.memset(sh1[:, :], 0.0)
            P.memset(sc1[:, :], 0.0)
        P.ts(sc1[:, :], sc1[:, :], 1.0, None, ALU.add)
        xt = [P.sbuf(f"x0_{i}", [128, D], F32) for i in range(2)]
        xn = [P.sbuf(f"xn0_{i}", [128, D], BF16) for i in range(2)]
        ho = [P.sbuf(f"ho_{i}", [128, 16, 128], BF16) for i in range(2)]
        st = P.sbuf("st0", [128, 24], F32)
        mv = P.sbuf("mv0", [128, 8], F32)
        ptb = [P.psum(f"ptb{i}", [128, 4, 128], BF16) for i in range(2)] if False else None
        hTv = self.hT.rearrange("(c p) s -> p c s", p=128)
        for t in range(self.NT if BIS & 4 else 0):
            x_ = xt[t % 2]
            P.dma(x_[:, :], self.x[t * 128:(t + 1) * 128, :])
            self.ln_stats(x_, st, mv, mv[:, 4:5], mv[:, 5:6], LN_EPS)
            xn_ = xn[t % 2]
            P.act(xn_[:, :], x_[:, :], AF.Identity, bias=mv[:, 5:6], scale=mv[:, 4:5])
            h_ = ho[t % 2]
            for c4 in range(4):
                pb = self.qb()
                for k in range(4):
                    cc = c4 * 4 + k
                    P.tr(pb[:, k * 128:(k + 1) * 128], xn_[:, cc * 128:(cc + 1) * 128], self.identb[:, :])
                for k in range(4):
                    cc = c4 * 4 + k
                    P.ts(h_[:, cc, :], pb[:, k * 128:(k + 1) * 128], sc1[:, cc:cc + 1], sh1[:, cc:cc + 1], ALU.mult, ALU.add)
            if BIS & 16:
                P.dma(hTv[:, :, t * 128:(t + 1) * 128], h_[:, :, :], wr=[P.uniq("hT")])
        P.release(mk)

    def qb(self):
        b = self.bfb[self.qbi % len(self.bfb)]
        self.qbi += 1
        return b

    def phaseA(self):
        P, nc, S = self.P, self.nc, self.S
        mk = P.mark()
        self.set_psum(8, 0)
        NTL = S // 512
        hTv = self.hT.rearrange("(c p) s -> p c s", p=128)
        hbuf = [P.sbuf(f"hA{i}", [128, 16, 512], BF16) for i in range(2)]
        wbuf = [P.sbuf(f"wA{i}", [128, 16, 772], BF16) for i in range(2)]
        mb = P.sbuf("mb", [128, 16], F32)
        P.dma(mb[:, :], self.m_bias[0:1, :].partition_broadcast(128))
        P.ts(mb[:, 0:8], mb[:, 0:8], 1.0 / 15.0, None, ALU.mult)
        gab = P.sbuf("gab", [128, 64], F32)
        P.dma(gab[:, :], self.g_ab[0:1, :].partition_broadcast(128))
        P.act(gab[:, 0:32], gab[:, 0:32], AF.Exp)
        P.ts(gab[:, 0:32], gab[:, 0:32], -1.0, None, ALU.mult)
        gnw = P.sbuf("gnw", [128, 128], F32)
        P.dma(gnw[:, :], self.g_norm_w[0:1, :].partition_broadcast(128))
        mnw = P.sbuf("mnw", [128, 256], F32)
        cw = P.sbuf("cw", [128, 16], F32)

        W = {}

        def wt(name, shape=(128, 128), dt=F32, n=2):
            W[name] = [P.sbuf(f"A_{name}{i}", list(shape), dt) for i in range(n)]

        for nm in ["Fm", "fbc", "DmT", "DmTm", "eb", "SmT", "QdT", "kd", "ktm", "dec", "decT", "eGb", "decmb",
                   "decTm", "attnT", "Nm", "NT", "T", "R", "Y", "tmp", "vb", "kbg", "kdec", "nwT", "vnew", "sz", "t1"]:
            wt(nm)
        wt("v1", (128, 257))
        wt("tk3", (128, 260))
        wt("KK")
        wt("AT")
        wt("hraw", (128, 256))
        wt("sig", (128, 256))
        wt("junk", (128, 256))
        wt("hm", (128, 256), BF16)
        wt("on", (128, 128), BF16)
        wt("col", (128, 16), n=4)
        self.wi = {k: 0 for k in W}

        def g(name):
            i = self.wi[name]
            self.wi[name] = i + 1
            return W[name][i % len(W[name])]

        qT_all = P.sbuf("qT_all", [128, 512], F32)
        kT_all = P.sbuf("kT_all", [128, 512], F32)
        Cn = P.sbuf("Cn", [128, 257], F32)
        for v in W["v1"]:
            P.memset(v[:, 256:257], 1.0)

        load_n = [0]

        def load_h(T):
            hb = hbuf[load_n[0] % 2]
            load_n[0] += 1
            P.dma(hb[:, :, :], hTv[:, :, T * 512:(T + 1) * 512], rd=[P.uniq("hTr")])
            return hb

        for hd in range(8):
            wm = wbuf[hd % 2]
            P.dma(wm[:, :, 0:770], self.w_in_m[hd].rearrange("p (c n) -> p c n", c=16), eng="pool")
            P.dma(mnw[:, :], self.m_norm_w[0:1, hd * 256:(hd + 1) * 256].partition_broadcast(128))
            P.memset(Cn[:, :], 0.0)
            hb_next = load_h(0)
            for T in range(NTL):
                hb = hb_next
                if T + 1 < NTL:
                    hb_next = load_h(T + 1)
                for ct, dst, sc in ((0, qT_all, 1.0), (1, kT_all, 128.0 ** -0.5)):
                    ps = self.bank()
                    for cc in range(16):
                        P.mm(ps[:, :], wm[:, cc, ct * 128:(ct + 1) * 128], hb[:, cc, :], start=(cc == 0), stop=(cc == 15))
                    P.act(dst[:, :], ps[:, :], AF.Copy, scale=sc)
                for j in range(4):
                    tok0 = T * 512 + j * 128
                    ps1 = self.bank()
                    ps2 = self.bank()
                    for cc in range(16):
                        P.mm(ps1[:, 0:386], hb[:, cc, j * 128:(j + 1) * 128], wm[:, cc, 128:514], start=(cc == 0), stop=(cc == 15))
                    for cc in range(16):
                        P.mm(ps2[:, 0:256], hb[:, cc, j * 128:(j + 1) * 128], wm[:, cc, 514:770], start=(cc == 0), stop=(cc == 15))
                    qT = qT_all[:, j * 128:(j + 1) * 128]
                    kT = kT_all[:, j * 128:(j + 1) * 128]
                    col = g("col")
                    ktm = g("ktm")
                    P.act(ktm[:, :], ps1[:, 0:128], AF.Copy, scale=128.0 ** -0.5)
                    v1 = g("v1")
                    P.copy(v1[:, 0:256], ps1[:, 128:384])
                    sig = g("sig")
                    P.act(sig[:, :], ps2[:, 0:256], AF.Sigmoid)
                    P.copy(col[:, 12:14], ps1[:, 384:386])
                    P.act(col[:, 0:1], col[:, 12:13], AF.Tanh, bias=mb[:, hd:hd + 1], scale=1.0 / 15.0)
                    P.ts(col[:, 1:2], col[:, 0:1], 15.0, None, ALU.mult)
                    P.ts(col[:, 2:3], col[:, 13:14], mb[:, 8 + hd:9 + hd], None, ALU.add)
                    self.softplus_parts(col[:, 2:3], col[:, 3:4], col[:, 4:5], col[:, 5:6])
                    P.stt(col[:, 6:7], col[:, 2:3], 0.0, col[:, 5:6], ALU.min, ALU.subtract)
                    Fm = g("Fm")
                    fbc = g("fbc")
                    P.ts(Fm[:, :], self.GT, col[:, 6:7], None, ALU.mult)
                    P.ts(fbc[:, :], self.ones, col[:, 6:7], None, ALU.mult)
                    psD = self.q()
                    psE = self.q()
                    P.mm(psD, Fm[:, :], self.U)
                    P.mm(psE, fbc[:, :], self.U)
                    DmT = g("DmT")
                    P.act(DmT[:, :], psD, AF.Exp, bias=col[:, 1:2])
                    DmTm = g("DmTm")
                    P.tt(DmTm[:, :], DmT[:, :], self.U, ALU.mult)
                    eb = g("eb")
                    P.act(eb[:, :], psE, AF.Exp)
                    psS = self.q()
                    P.mm(psS, kT, qT)
                    SmT = g("SmT")
                    P.tt(SmT[:, :], psS, DmTm[:, :], ALU.mult)
                    QdT = g("QdT")
                    P.tt(QdT[:, :], qT, eb[:, :], ALU.mult)
                    psN = self.bank()
                    P.mm(psN[:, 0:257], QdT[:, :], Cn[:, :], start=True, stop=False)
                    P.mm(psN[:, 0:257], SmT[:, :], v1[:, :], start=False, stop=True)
                    kd = g("kd")
                    P.ts(kd[:, :], ktm[:, :], DmT[:, 127:128], None, ALU.mult)
                    psC = self.bank()
                    P.mm(psC[:, 0:257], kd[:, :], v1[:, :])
                    P.stt(Cn[:, :], Cn[:, :], eb[:, 127:128], psC[:, 0:257], ALU.mult, ALU.add)
                    P.copy(col[:, 14:15], psN[:, 256:257])
                    P.stt(col[:, 7:8], col[:, 14:15], -1.0, col[:, 14:15], ALU.mult, ALU.max)
                    P.ts(col[:, 7:8], col[:, 7:8], 1.0, None, ALU.max)
                    P.recip(col[:, 8:9], col[:, 7:8])
                    hraw = g("hraw")
                    P.ts(hraw[:, :], psN[:, 0:256], col[:, 8:9], None, ALU.mult)
                    junk = g("junk")
                    P.act(junk[:, :], hraw[:, :], AF.Square, accum_out=col[:, 9:10])
                    self.rstd_from_ss(col[:, 9:10], 256.0, col[:, 10:11], col[:, 11:12])
                    t1 = g("junk")
                    P.stt(t1[:, :], hraw[:, :], col[:, 11:12], mnw[:, :], ALU.mult, ALU.mult)
                    hm = g("hm")
                    P.tt(hm[:, :], t1[:, :], sig[:, :], ALU.mult)
                    P.dma(self.mix[tok0:tok0 + 128, hd * 256:(hd + 1) * 256], hm[:, :], wr=[P.uniq("mix")])

        xc = P.sbuf("xc", [128, 4, 515], F32)
        cv = P.sbuf("cv", [128, 4, 512], F32)
        cs = P.sbuf("cs", [128, 4, 512], F32)
        sq = P.sbuf("sq", [128, 2, 512], F32)
        rn = P.sbuf("rn", [128, 2, 512], F32)
        Sst = [P.sbuf(f"Sst{i}", [128, 128], F32) for i in range(2)]
        for jh in range(16):
            wg = wbuf[jh % 2]
            P.dma(wg[:, :, :].rearrange("p c n -> p (c n)"), self.w_in_g[jh], eng="pool", max_dma_last_dim=8192)
            P.dma(cw[:, :], self.conv_g[jh])
            P.memset(xc[:, :, 0:3], 0.0)
            for s_ in Sst:
                P.memset(s_[:, :], 0.0)
            hb_next = load_h(0)
            for T in range(NTL):
                hb = hb_next
                if T + 1 < NTL:
                    hb_next = load_h(T + 1)
                for ct in range(4):
                    ps = self.bank()
                    for cc in range(16):
                        P.mm(ps[:, :], wg[:, cc, ct * 128:(ct + 1) * 128], hb[:, cc, :], start=(cc == 0), stop=(cc == 15))
                    P.copy(xc[:, ct, 3:515], ps[:, :], eng="act")
                for ct in range(4):
                    P.ts(cv[:, ct, :], xc[:, ct, 0:512], cw[:, ct * 4:ct * 4 + 1], None, ALU.mult)
                    for k in range(1, 4):
                        P.stt(cv[:, ct, :], xc[:, ct, k:k + 512], cw[:, ct * 4 + k:ct * 4 + k + 1], cv[:, ct, :], ALU.mult, ALU.add)
                P.copy(xc[:, :, 0:3], xc[:, :, 512:515])
                P.act(cs[:, :, :], cv[:, :, :], AF.Silu)
                P.act(sq[:, :, :], cs[:, 0:2, :], AF.Square)
                for ct in range(2):
                    ps = self.bank()
                    P.mm(ps[:, :], self.ones, sq[:, ct, :])
                    P.ts(rn[:, ct, :], ps[:, :], RMS_EPS, None, ALU.add)
                P.act(rn[:, :, :], rn[:, :, :], AF.Sqrt)
                P.recip(rn[:, :, :], rn[:, :, :])
                P.stt(qT_all[:, :], cs[:, 0, :], 128.0 ** -0.5, rn[:, 0, :], ALU.mult, ALU.mult)
                P.tt(kT_all[:, :], cs[:, 1, :], rn[:, 1, :], ALU.mult)
                for j in range(4):
                    tok0 = T * 512 + j * 128
                    sl = slice(j * 128, (j + 1) * 128)
                    ps3 = self.bank()
                    for cc in range(16):
                        P.mm(ps3[:, 0:260], hb[:, cc, sl], wg[:, cc, 512:772], start=(cc == 0), stop=(cc == 15))
                    qT = qT_all[:, sl]
                    kT = kT_all[:, sl]
                    psK = self.q()
                    P.tr(psK, kT, self.ident)
                    ktm = g("ktm")
                    P.copy(ktm[:, :], psK, eng="act")
                    tk3 = g("tk3")
                    P.copy(tk3[:, :], ps3[:, 0:260], eng="act")
                    ps3 = tk3
                    psKK_ = self.q()
                    P.mm(psKK_, kT, kT)
                    psKK = g("KK")
                    P.copy(psKK[:, :], psKK_, eng="act")
                    psKK = psKK[:, :]
                    psAT_ = self.q()
                    P.mm(psAT_, kT, qT)
                    psAT = g("AT")
                    P.copy(psAT[:, :], psAT_)
                    psAT = psAT[:, :]
                    def chain(hv, ps3=ps3, psKK=psKK, psAT=psAT, qT=qT, kT=kT, ktm=ktm, tok0=tok0, sl=sl):
                        hh = 2 * jh + hv
                        col = g("col")
                        P.act(col[:, 0:1], ps3[:, 2 + hv:3 + hv], AF.Sigmoid)
                        P.ts(col[:, 1:2], ps3[:, hv:hv + 1], gab[:, 32 + hh:33 + hh], None, ALU.add)
                        self.softplus_parts(col[:, 1:2], col[:, 2:3], col[:, 3:4], col[:, 4:5])
                        P.stt(col[:, 5:6], col[:, 1:2], 0.0, col[:, 4:5], ALU.max, ALU.add)
                        P.ts(col[:, 6:7], col[:, 5:6], gab[:, hh:hh + 1], None, ALU.mult)
                        yield
                        Fm = g("Fm")
                        gbc = g("fbc")
                        P.ts(Fm[:, :], self.GT, col[:, 6:7], None, ALU.mult)
                        P.ts(gbc[:, :], self.ones, col[:, 6:7], None, ALU.mult)
                        psD = self.q()
                        psDT = self.q()
                        psEG = self.q()
                        psG = self.q()
                        P.mm(psD, self.U, Fm[:, :])
                        P.mm(psDT, Fm[:, :], self.U)
                        P.mm(psEG, gbc[:, :], self.U)
                        P.mm(psG[:, 0:1], self.U, col[:, 6:7])
                        dec = g("dec")
                        decT = g("decT")
                        eGb = g("eGb")
                        P.act(dec[:, :], psD, AF.Exp)
                        P.act(decT[:, :], psDT, AF.Exp)
                        P.act(eGb[:, :], psEG, AF.Exp)
                        P.act(col[:, 7:8], psG[:, 0:1], AF.Exp)
                        decmb = g("decmb")
                        P.stt(decmb[:, :], dec[:, :], col[:, 0:1], self.GT, ALU.mult, ALU.mult)
                        Nm = g("Nm")
                        P.tt(Nm[:, :], psKK, decmb[:, :], ALU.mult)
                        decTm = g("decTm")
                        P.tt(decTm[:, :], decT[:, :], self.U, ALU.mult)
                        attnT = g("attnT")
                        P.tt(attnT[:, :], psAT, decTm[:, :], ALU.mult)
                        QdT = g("QdT")
                        P.tt(QdT[:, :], qT, eGb[:, :], ALU.mult)
                        yield
                        psVt = self.q()
                        P.tr(psVt, cs[:, 2 + hv, sl], self.ident)
                        vb = g("vb")
                        P.ts(vb[:, :], psVt, col[:, 0:1], None, ALU.mult)
                        P.tt(col[:, 8:9], col[:, 0:1], col[:, 7:8], ALU.mult)
                        kbg = g("kbg")
                        P.ts(kbg[:, :], ktm[:, :], col[:, 8:9], None, ALU.mult)
                        kdec = g("kdec")
                        P.ts(kdec[:, :], ktm[:, :], decT[:, 127:128], None, ALU.mult)
                        yield
                        psNT = self.q()
                        P.tr(psNT, Nm[:, :], self.ident)
                        NT_ = g("NT")
                        P.copy(NT_[:, :], psNT, eng="act")
                        tmp = g("tmp")
                        P.tt(tmp[:, :], Nm[:, :], self.negm[0], ALU.mult)
                        Tc = g("T")
                        P.tt(Tc[:, :], tmp[:, :], self.ident, ALU.add)
                        psR = self.q()
                        P.tr(psR, Tc[:, :], self.ident)
                        Rc = g("R")
                        P.copy(Rc[:, :], psR, eng="act")
                        yield
                        for l in range(1, 7):
                            psY = self.q()
                            P.mm(psY, NT_[:, :], Tc[:, :])
                            Y = g("Y")
                            P.copy(Y[:, :], psY, eng="act")
                            yield
                            psZ = self.q()
                            P.mm(psZ, Rc[:, :], Y[:, :])
                            tmp = g("tmp")
                            P.tt(tmp[:, :], psZ, self.negm[l], ALU.mult)
                            Tn = g("T")
                            P.tt(Tn[:, :], tmp[:, :], Tc[:, :], ALU.add)
                            Tc = Tn
                            yield
                            psR = self.q()
                            P.tr(psR, Tc[:, :], self.ident)
                            Rc = g("R")
                            P.copy(Rc[:, :], psR, eng="act")
                            yield
                        psW = self.q()
                        P.mm(psW, kbg[:, :], Rc[:, :])
                        nwT = g("nwT")
                        P.act(nwT[:, :], psW, AF.Copy, scale=-1.0)
                        yield
                        Sh = Sst[hv]
                        psV = self.q()
                        P.mm(psV, Rc[:, :], vb[:, :], start=True, stop=False)
                        P.mm(psV, nwT[:, :], Sh[:, :], start=False, stop=True)
                        vnew = g("vnew")
                        P.copy(vnew[:, :], psV, eng="act")
                        yield
                        psO = self.q()
                        P.mm(psO, QdT[:, :], Sh[:, :], start=True, stop=False)
                        P.mm(psO, attnT[:, :], vnew[:, :], start=False, stop=True)
                        psS = self.q()
                        P.mm(psS, kdec[:, :], vnew[:, :])
                        P.stt(Sh[:, :], Sh[:, :], eGb[:, 127:128], psS, ALU.mult, ALU.add)
                        yield
                        junk = g("tmp")
                        P.act(junk[:, :], psO, AF.Square, accum_out=col[:, 9:10])
                        self.rstd_from_ss(col[:, 9:10], 128.0, col[:, 10:11], col[:, 11:12])
                        sz = g("sz")
                        P.act(sz[:, :], ps3[:, 4 + hv * 128:4 + (hv + 1) * 128], AF.Silu)
                        t1 = g("t1")
                        P.stt(t1[:, :], psO, col[:, 11:12], gnw[:, :], ALU.mult, ALU.mult)
                        on = g("on")
                        P.tt(on[:, :], t1[:, :], sz[:, :], ALU.mult)
                        P.dma(self.mix[tok0:tok0 + 128, D + hh * 128:D + (hh + 1) * 128], on[:, :], wr=[P.uniq("mix")])
                    gens = [chain(0), chain(1)]
                    while gens:
                        for gn in list(gens):
                            try:
                                next(gn)
                            except StopIteration:
                                gens.remove(gn)
        P.release(mk)

    def phaseB(self):
        P, nc, S, NE = self.P, self.nc, self.S, self.NE
        TB = 256
        mk = P.mark()
        self.set_psum(5, 3)
        hTv = self.hT.rearrange("(c p) s -> p c s", p=128)
        mixtm = P.sbuf("mixtm", [128, 2, 3 * D], BF16)
        mixT = P.sbuf("mixT", [128, 48, TB], BF16)
        hTt = P.sbuf("hTB", [128, 16, TB], BF16)
        xt = P.sbuf("xB", [128, 2, D], F32)
        wa = [P.sbuf(f"waB{i}", [128, 16, 128], BF16) for i in range(2)]
        wb = [P.sbuf(f"wbB{i}", [128, 32, 128], BF16) for i in range(2)]
        wr = [P.sbuf(f"wrB{i}", [128, 16, 256], BF16) for i in range(2)]
        wo = [P.sbuf(f"woB{i}", [128, 16, 512], BF16) for i in range(2)]
        mT = P.sbuf("mergedT", [128, 16, TB], BF16)
        pre = P.sbuf("preB", [128, 2, D], F32)
        g1b = P.sbuf("g1b", [128, D], F32)
        sa = [P.sbuf(f"saB{i}", [128, TB], F32) for i in range(2)]
        sb_ = [P.sbuf(f"sbB{i}", [128, TB], F32) for i in range(2)]
        P.dma(g1b[:, :], self.modrow[0:1, 2 * D:3 * D].partition_broadcast(128))
        wn = 0
        for tb in range(S // TB):
            t0 = tb * TB
            P.dma(mixtm[:, :, :], self.mix[t0:t0 + TB, :].rearrange("(s p) f -> p s f", p=128), rd=[P.uniq("mixr")])
            P.dma(hTt[:, :, :], hTv[:, :, t0:t0 + TB], rd=[P.uniq("hTr")])
            P.dma(xt[:, :, :], self.x[t0:t0 + TB, :].rearrange("(s p) f -> p s f", p=128))
            k = 0
            for s in range(2):
                for f4 in range(12):
                    pb = self.qb()
                    for kk in range(4):
                        fc = f4 * 4 + kk
                        P.tr(pb[:, kk * 128:(kk + 1) * 128], mixtm[:, s, fc * 128:(fc + 1) * 128], self.identb[:, :])
                    P.copy(mixT[:, f4 * 4:f4 * 4 + 4, s * 128:(s + 1) * 128], pb[:, 0:512].rearrange("p (k t) -> p k t", k=4),
                           eng=("act" if k % 2 else "dve"))
                    k += 1
            for n in range(16):
                a_, b_, r_ = wa[wn % 2], wb[wn % 2], wr[wn % 2]
                wn += 1
                P.dma(a_[:, :, :].rearrange("p c n -> p (c n)"), self.w_a[n], eng="pool", max_dma_last_dim=8192)
                P.dma(b_[:, :, :].rearrange("p c n -> p (c n)"), self.w_b[n], eng="pool", max_dma_last_dim=8192)
                P.dma(r_[:, :, :].rearrange("p c n -> p (c n)"), self.w_rarb[n], eng="pool", max_dma_last_dim=8192)
                pA, pB, pRa, pRb = self.bank(), self.bank(), self.bank(), self.bank()
                for cc in range(16):
                    P.mm(pA[:, 0:TB], a_[:, cc, :], mixT[:, cc, :], start=(cc == 0), stop=(cc == 15))
                for cc in range(32):
                    P.mm(pB[:, 0:TB], b_[:, cc, :], mixT[:, 16 + cc, :], start=(cc == 0), stop=(cc == 31))
                for cc in range(16):
                    P.mm(pRa[:, 0:TB], r_[:, cc, 0:128], hTt[:, cc, :], start=(cc == 0), stop=(cc == 15))
                for cc in range(16):
                    P.mm(pRb[:, 0:TB], r_[:, cc, 128:256], hTt[:, cc, :], start=(cc == 0), stop=(cc == 15))
                s1, s2 = sa[n % 2], sb_[n % 2]
                P.act(s1[:, :], pRa[:, 0:TB], AF.Sigmoid)
                P.act(s2[:, :], pRb[:, 0:TB], AF.Sigmoid)
                P.tt(s1[:, :], pA[:, 0:TB], s1[:, :], ALU.mult)
                P.tt(s2[:, :], pB[:, 0:TB], s2[:, :], ALU.mult)
                P.tt(mT[:, n, :], s1[:, :], s2[:, :], ALU.add)
            for cb in range(4):
                o_ = wo[cb % 2]
                P.dma(o_[:, :, :].rearrange("p c n -> p (c n)"), self.w_out[cb], eng="pool", max_dma_last_dim=8192)
                for s in range(2):
                    pM = self.bank()
                    for cc in range(16):
                        P.mm(pM[:, :], mT[:, cc, s * 128:(s + 1) * 128], o_[:, cc, :], start=(cc == 0), stop=(cc == 15))
                    dst = pre[:, s, cb * 512:(cb + 1) * 512]
                    P.tt(dst, pM[:, :], g1b[:, cb * 512:(cb + 1) * 512], ALU.mult)
                    P.stt(dst, xt[:, s, cb * 512:(cb + 1) * 512], ALPHA, dst, ALU.mult, ALU.add)
            P.dma(self.pre[t0:t0 + TB, :].rearrange("(s p) f -> p s f", p=128), pre[:, :, :], wr=[P.uniq("prew")])
        P.release(mk)

        mk = P.mark()
        self.set_psum(8, 0)
        lnb = [P.sbuf(f"lnb{i}", [128, D], F32) for i in range(4)]
        P.dma(lnb[0][:, :], self.lnp[0:1, :].partition_broadcast(128))
        P.dma(lnb[1][:, :], self.lnp[1:2, :].partition_broadcast(128))
        P.dma(lnb[3][:, :], self.modrow[0:1, 3 * D:4 * D].partition_broadcast(128))
        P.dma(lnb[2][:, :], self.modrow[0:1, 4 * D:5 * D].partition_broadcast(128))
        P.ts(lnb[2][:, :], lnb[2][:, :], 1.0, None, ALU.add)
        wrt = P.sbuf("wrt", [128, 16, NE], F32)
        P.dma(wrt[:, :, :], self.w_router.rearrange("(c p) n -> p c n", p=128))
        brb = P.sbuf("brb", [128, NE], F32)
        P.dma(brb[:, :], self.b_router[0:1, :].partition_broadcast(128))
        pt_ = [P.sbuf(f"pre2_{i}", [128, D], F32) for i in range(2)]
        x1t = [P.sbuf(f"x1t_{i}", [128, D], F32) for i in range(2)]
        h2t = [P.sbuf(f"h2t_{i}", [128, D], F32) for i in range(2)]
        h2Tf = [P.sbuf(f"h2Tf_{i}", [128, 16, 128], F32) for i in range(2)]
        h2Tb = [P.sbuf(f"h2Tb_{i}", [128, 16, 128], BF16) for i in range(2)]
        st = P.sbuf("stB", [128, 24], F32)
        mv = P.sbuf("mvB", [128, 8], F32)
        lg = [P.sbuf(f"lg{i}", [128, 3, NE], F32) for i in range(2)]
        m8 = P.sbuf("m8", [128, 16], F32)
        h2Tv = self.h2T.rearrange("(c p) s -> p c s", p=128)
        for t in range(self.NT):
            t0 = t * 128
            p_, x_, h_ = pt_[t % 2], x1t[t % 2], h2t[t % 2]
            P.dma(p_[:, :], self.pre[t0:t0 + 128, :], rd=[P.uniq("prer")])
            self.ln_stats(p_, st, mv, mv[:, 4:5], mv[:, 5:6], LN_EPS)
            P.act(x_[:, :], p_[:, :], AF.Identity, bias=mv[:, 5:6], scale=mv[:, 4:5])
            P.tt(x_[:, :], x_[:, :], lnb[0][:, :], ALU.mult)
            P.tt(x_[:, :], x_[:, :], lnb[1][:, :], ALU.add)
            P.dma(self.x1[t0:t0 + 128, :], x_[:, :], wr=[P.uniq("x1w")])
            self.ln_stats(x_, st, mv, mv[:, 4:5], mv[:, 5:6], LN_EPS)
            P.act(h_[:, :], x_[:, :], AF.Identity, bias=mv[:, 5:6], scale=mv[:, 4:5])
            P.tt(h_[:, :], h_[:, :], lnb[2][:, :], ALU.mult)
            P.tt(h_[:, :], h_[:, :], lnb[3][:, :], ALU.add)
            hf, hb = h2Tf[t % 2], h2Tb[t % 2]
            for cc in range(16):
                pq = self.q()
                P.tr(pq, h_[:, cc * 128:(cc + 1) * 128], self.ident)
                P.copy(hf[:, cc, :], pq, eng="act")
                P.copy(hb[:, cc, :], pq, eng="dve")
            P.dma(h2Tv[:, :, t0:t0 + 128], hb[:, :, :], wr=[P.uniq("h2Tw")])
            pl = self.q()
            for cc in range(16):
                P.mm(pl[:, 0:NE], hf[:, cc, :], wrt[:, cc, :], start=(cc == 0), stop=(cc == 15))
            l_ = lg[t % 2]
            P.tt(l_[:, 0, :], pl[:, 0:NE], brb[:, :], ALU.add)
            P.add("dve", (lambda l_=l_: nc.vector.max(m8[:, 0:8], l_[:, 0, :])), reads=[l_[:, 0, :]], writes=[m8[:, 0:8]])
            P.ts(l_[:, 1, :], l_[:, 0, :], m8[:, 3:4], None, ALU.is_ge)
            P.ts(m8[:, 8:9], m8[:, 0:1], -1.0, None, ALU.mult)
            P.act(l_[:, 2, :], l_[:, 0, :], AF.Exp, bias=m8[:, 8:9])
            P.tt(l_[:, 2, :], l_[:, 2, :], l_[:, 1, :], ALU.mult)
            P.add("dve", (lambda l_=l_: nc.vector.reduce_sum(m8[:, 9:10], l_[:, 2, :], mybir.AxisListType.X)),
                  reads=[l_[:, 2, :]], writes=[m8[:, 9:10]])
            P.recip(m8[:, 10:11], m8[:, 9:10])
            P.ts(l_[:, 0, :], l_[:, 2, :], m8[:, 10:11], None, ALU.mult)
            P.dma(self.gates_d[t0:t0 + 128, :], l_[:, 0, :], wr=[P.uniq("gw")])
        P.release(mk)

    def phaseC(self):
        P, nc, S, NE = self.P, self.nc, self.S, self.NE
        TG = min(1024, S)
        NS = TG // 128
        NH = TG // 512
        mk = P.mark()
        self.set_psum(8, 0)
        h2Tv = self.h2T.rearrange("(c p) s -> p c s", p=128)
        acc = P.sbuf("acc", [128, NS, D], F32)
        h2g = P.sbuf("h2g", [128, 16, TG], BF16)
        actT = P.sbuf("actT", [128, 16, TG], BF16)
        wu = [P.sbuf(f"wu{i}", [128, 16, 256], BF16) for i in range(2)]
        wd = [P.sbuf(f"wd{i}", [128, 16, 256], BF16) for i in range(2)]
        gt = P.sbuf("gt", [128, NS, NE], F32)
        gTs = P.sbuf("gTs", [NE, 128], F32)
        bd = P.sbuf("bd", [NE, D], F32)
        bg = P.sbuf("bg", [128, NE * 16], F32)
        bu = P.sbuf("bu", [128, NE * 16], F32)
        P.dma(bd[:, :], self.b_down[:, :])
        P.dma(bg[:, :], self.b_upg[:, :])
        P.dma(bu[:, :], self.b_upu[:, :])
        ew = [[P.sbuf(f"ew{k}_{i}", [128, 512], F32) for i in range(2)] for k in range(3)]
        wun = 0
        wdn = 0
        en = 0
        for G in range(S // TG):
            g0 = G * TG
            P.dma(h2g[:, :, :], h2Tv[:, :, g0:g0 + TG], rd=[P.uniq("h2Tr")])
            P.dma(gt[:, :, :], self.gates_d[g0:g0 + TG, :].rearrange("(s p) e -> p s e", p=128), rd=[P.uniq("gr")])
            for s in range(NS):
                pq = self.q()
                P.tr(pq[0:NE, :], gt[:, s, :], self.ident)
                P.copy(gTs[:, :], pq[0:NE, :], eng="act")
                for cb in range(4):
                    pb = self.bank()
                    P.mm(pb[:, :], gTs[:, :], bd[:, cb * 512:(cb + 1) * 512])
                    P.copy(acc[:, s, cb * 512:(cb + 1) * 512], pb[:, :], eng=("act" if cb % 2 else "dve"))
            for e in range(NE):
                for jt in range(16):
                    w_ = wu[wun % 2]
                    wun += 1
                    P.dma(w_[:, :, :].rearrange("p c n -> p (c n)"), self.w_ups[e // self.EG][e % self.EG, jt], eng="pool", max_dma_last_dim=8192)
                    for hf in range(NH):
                        tsl = slice(hf * 512, (hf + 1) * 512)
                        pG, pU = self.bank(), self.bank()
                        for cc in range(16):
                            P.mm(pG[:, :], w_[:, cc, 0:128], h2g[:, cc, tsl], start=(cc == 0), stop=(cc == 15))
                        for cc in range(16):
                            P.mm(pU[:, :], w_[:, cc, 128:256], h2g[:, cc, tsl], start=(cc == 0), stop=(cc == 15))
                        g_, s_, u_ = ew[0][en % 2], ew[1][en % 2], ew[2][en % 2]
                        en += 1
                        bi = e * 16 + jt
                        P.ts(g_[:, :], pG[:, :], bg[:, bi:bi + 1], 7.0, ALU.add, ALU.min)
                        P.act(s_[:, :], g_[:, :], AF.Sigmoid, scale=1.702)
                        P.ts(u_[:, :], pU[:, :], bu[:, bi:bi + 1], 7.0, ALU.add, ALU.min)
                        P.ts(u_[:, :], u_[:, :], -7.0, 1.0, ALU.max, ALU.add)
                        P.tt(g_[:, :], g_[:, :], s_[:, :], ALU.mult)
                        P.tt(actT[:, jt, tsl], g_[:, :], u_[:, :], ALU.mult)
                for cb in range(8):
                    d_ = wd[wdn % 2]
                    wdn += 1
                    P.dma(d_[:, :, :].rearrange("p c n -> p (c n)"), self.w_downs[e // self.EG][e % self.EG, cb], eng="pool", max_dma_last_dim=8192)
                    for s in range(NS):
                        pb = self.bank()
                        for cc in range(16):
                            P.mm(pb[:, 0:256], actT[:, cc, s * 128:(s + 1) * 128], d_[:, cc, :], start=(cc == 0), stop=(cc == 15))
                        dst = acc[:, s, cb * 256:(cb + 1) * 256]
                        P.stt(dst, pb[:, 0:256], gt[:, s, e:e + 1], dst, ALU.mult, ALU.add)
            P.dma(self.ffn[g0:g0 + TG, :].rearrange("(s p) f -> p s f", p=128), acc[:, :, :], wr=[P.uniq("ffnw")])
        P.release(mk)
        mk = P.mark()
        fb = [P.sbuf(f"fb{i}", [128, D], F32) for i in range(3)]
        P.dma(fb[0][:, :], self.modrow[0:1, 5 * D:6 * D].partition_broadcast(128))
        P.dma(fb[1][:, :], self.lnp[2:3, :].partition_broadcast(128))
        P.dma(fb[2][:, :], self.lnp[3:4, :].partition_broadcast(128))
        st = P.sbuf("stD", [128, 24], F32)
        mv = P.sbuf("mvD", [128, 8], F32)
        fa = [P.sbuf(f"fa{i}", [128, D], F32) for i in range(2)]
        x1b = [P.sbuf(f"x1b{i}", [128, D], F32) for i in range(2)]
        for t in range(self.NT):
            t0 = t * 128
            a_, x_ = fa[t % 2], x1b[t % 2]
            P.dma(a_[:, :], self.ffn[t0:t0 + 128, :], rd=[P.uniq("ffnr")])
            P.dma(x_[:, :], self.x1[t0:t0 + 128, :], rd=[P.uniq("x1r")])
            P.tt(a_[:, :], a_[:, :], fb[0][:, :], ALU.mult)
            P.stt(a_[:, :], x_[:, :], ALPHA, a_[:, :], ALU.mult, ALU.add)
            self.ln_stats(a_, st, mv, mv[:, 4:5], mv[:, 5:6], LN_EPS)
            P.act(x_[:, :], a_[:, :], AF.Identity, bias=mv[:, 5:6], scale=mv[:, 4:5])
            P.tt(x_[:, :], x_[:, :], fb[1][:, :], ALU.mult)
            P.tt(x_[:, :], x_[:, :], fb[2][:, :], ALU.add)
            P.dma(self.out[t0:t0 + 128, :], x_[:, :])
        P.release(mk)


def _prep_inputs(inputs, b, S, NE, shared=None):
    f = lambda a: np.ascontiguousarray(a, dtype=np.float32)
    if shared is not None:
        d = dict(shared)
        d["x"] = f(inputs["x"][b][:S])
        d["c"] = f(inputs["c"][b].reshape(128, 16))
        return d
    w_in = inputs["w_in"][0]
    wm = []
    for hd in range(8):
        cols = np.r_[hd * 128:(hd + 1) * 128, 1024 + hd * 128:1024 + (hd + 1) * 128,
                     2048 + hd * 256:2048 + (hd + 1) * 256, 4096 + hd, 4104 + hd,
                     4112 + hd * 256:4112 + (hd + 1) * 256]
        wm.append(w_in[:, cols])
    wg = []
    G0 = 6160
    for j in range(16):
        cols = np.r_[G0 + j * 128:G0 + (j + 1) * 128, G0 + 2048 + j * 128:G0 + 2048 + (j + 1) * 128,
                     G0 + 4096 + 2 * j * 128:G0 + 4096 + (2 * j + 2) * 128,
                     14352 + 2 * j:14352 + 2 * j + 2, 14384 + 2 * j:14384 + 2 * j + 2,
                     14416 + 2 * j * 128:14416 + (2 * j + 2) * 128]
        wg.append(w_in[:, cols])
    wrr = [np.concatenate([w_in[:, 18512 + n * 128:18512 + (n + 1) * 128],
                           w_in[:, 20560 + n * 128:20560 + (n + 1) * 128]], axis=1) for n in range(16)]
    conv = inputs["conv_w"][0]
    cg = []
    for j in range(16):
        tiles = [conv[:, j * 128:(j + 1) * 128], conv[:, 2048 + j * 128:2048 + (j + 1) * 128],
                 conv[:, 4096 + 2 * j * 128:4096 + (2 * j + 1) * 128], conv[:, 4096 + (2 * j + 1) * 128:4096 + (2 * j + 2) * 128]]
        cg.append(np.concatenate([t.T for t in tiles], axis=1))
    w_up = inputs["w_up"][0][:NE]
    w_up_r = w_up.reshape(NE, 16, 128, 16, 128, 2).transpose(0, 3, 2, 1, 5, 4).reshape(NE, 16, 128, 4096)
    w_down_r = inputs["w_down"][0][:NE].reshape(NE, 16, 128, 8, 256).transpose(0, 3, 2, 1, 4).reshape(NE, 8, 128, 4096)

    def pc(a, ncol):
        kc = a.shape[0] // 128
        return a.reshape(kc, 128, ncol).transpose(1, 0, 2).reshape(128, kc * ncol)
    b_up = inputs["b_up"][0][:NE].reshape(NE, 16, 128, 2)
    EG = min(8, NE)
    extra = {}
    for i in range(NE // EG):
        extra[f"w_up{i}"] = f(w_up_r[i * EG:(i + 1) * EG])
        extra[f"w_down{i}"] = f(w_down_r[i * EG:(i + 1) * EG])
    return {
        **extra,
        "x": f(inputs["x"][b][:S]),
        "c": f(inputs["c"][b].reshape(128, 16)),
        "w_ada": f(inputs["w_ada"][0]),
        "b_ada": f(inputs["b_ada"][0][None, :]),
        "w_in_m": f(np.stack([pc(a, 770) for a in wm])),
        "w_in_g": f(np.stack([pc(a, 772) for a in wg])),
        "w_rarb": f(np.stack([pc(a, 256) for a in wrr])),
        "m_bias": f(np.concatenate([inputs["m_bias_i"][0], inputs["m_bias_f"][0]])[None, :]),
        "m_norm_w": f(inputs["m_norm_w"][0][None, :]),
        "conv_g": f(np.stack(cg)),
        "g_ab": f(np.concatenate([inputs["g_a_log"][0], inputs["g_dt_bias"][0]])[None, :]),
        "g_norm_w": f(inputs["g_norm_w"][0][None, :]),
        "w_a": f(np.stack([pc(inputs["w_branch_a"][0][:, n * 128:(n + 1) * 128], 128) for n in range(16)])),
        "w_b": f(np.stack([pc(inputs["w_branch_b"][0][:, n * 128:(n + 1) * 128], 128) for n in range(16)])),
        "w_out": f(np.stack([pc(inputs["w_out"][0][:, cb * 512:(cb + 1) * 512], 512) for cb in range(4)])),
        "lnp": f(np.stack([inputs["ln1_g"][0], inputs["ln1_b"][0], inputs["ln2_g"][0], inputs["ln2_b"][0]])),
        "w_router": f(inputs["w_router"][0][:, :NE]),
        "b_router": f(inputs["b_router"][0][None, :NE]),
        "b_upg": f(b_up[:, :, :, 0].transpose(2, 0, 1).reshape(128, NE * 16)),
        "b_upu": f(b_up[:, :, :, 1].transpose(2, 0, 1).reshape(128, NE * 16)),
        "b_down": f(inputs["b_down"][0][:NE]),
        "consts": make_consts(),
    }


def run(inputs, S=4096, NE=32, cores=(0, 1), debug=False, phases="0ABC", trace=False):
    bld = Builder(S, NE, debug=debug, phases=phases)
    first = _prep_inputs(inputs, cores[0], S, NE)
    in_maps = [{k: v for k, v in _prep_inputs(inputs, b, S, NE, shared=first).items() if k in bld.decl} for b in cores]
    res = run_bass_kernel_spmd(bld.nc, in_maps, core_ids=list(range(len(cores))), trace=trace)
    return res, bld


def kernel(**inputs):
    inputs = {k: np.asarray(v) for k, v in inputs.items()}
    res, _ = run(inputs)
    return np.stack([np.asarray(r["out"], dtype=np.float32) for r in res.results], axis=0)
```

```python
import numpy as np
import concourse.bass as bass
import concourse.mybir as mybir
from concourse.bass_utils import run_bass_kernel_spmd

F32 = mybir.dt.float32
BF16 = mybir.dt.bfloat16
AF = mybir.ActivationFunctionType
ALU = mybir.AluOpType

SEM_PHASE = 12000
DMA_POOL = 24

D = 2048
DC = 16
IN_W = 22608
LN_EPS = 1e-5
RMS_EPS = 1e-6
ALPHA = 2.0 ** 0.25
NCONST = 11 * 128


class Ins:
    __slots__ = ("eng", "fn", "reads", "writes", "dma", "idx", "deps", "sig", "semv", "name")


class Prog:
    def __init__(self, nc):
        self.nc = nc
        self.ins = []
        self.tinfo = {}
        self.engs = {"pe": nc.tensor, "act": nc.scalar, "dve": nc.vector, "pool": nc.gpsimd, "sp": nc.sync}
        self._ctx = []
        self.self_sync = {"dve", "act", "pool"}
        self.uid = 0

    def sbuf(self, name, shape, dtype):
        g = self.nc.sbuf_tensor(name, list(shape), dtype)
        t = g.__enter__()
        self._ctx.append(g)
        self.tinfo[t.name] = (int(np.prod(shape[1:])) * mybir.dt.size(dtype), "sb")
        return t

    def psum(self, name, shape, dtype):
        g = self.nc.psum_tensor(name, list(shape), dtype)
        t = g.__enter__()
        self._ctx.append(g)
        self.tinfo[t.name] = (int(np.prod(shape[1:])) * mybir.dt.size(dtype), "ps")
        return t

    def dram(self, name, shape, dtype, kind="Internal"):
        t = self.nc.dram_tensor(name, list(shape), dtype, kind=kind)
        self.tinfo[t.name] = (None, "dr")
        return t

    def mark(self):
        return len(self._ctx)

    def release(self, mark):
        self.barrier()
        while len(self._ctx) > mark:
            g = self._ctx.pop()
            g.__exit__(None, None, None)

    def uniq(self, tag):
        self.uid += 1
        return (tag, 0, 1, self.uid, self.uid + 1)

    def region(self, ap):
        name = ap.tensor.name
        info = self.tinfo.get(name)
        if info is None:
            return None
        fe, sp = info
        pat = ap.ap
        es = mybir.dt.size(ap.dtype)
        off = int(ap.offset) * es
        if sp == "dr":
            ext = es
            for st, cnt in pat:
                ext += (cnt - 1) * abs(st) * es
            return (name, 0, 1, off, off + ext)
        if sp == "ps":
            return (name, 0, 128, 0, fe)
        pst, pcnt = pat[0]
        pst *= es
        p0 = off // fe
        f0 = off % fe
        pstep = max(1, pst // fe) if pst else 0
        p1 = p0 + (pcnt - 1) * pstep + 1
        ext = es
        for st, cnt in pat[1:]:
            ext += (cnt - 1) * abs(st) * es
        return (name, p0, p1, f0, f0 + ext)

    def add(self, eng, fn, reads=(), writes=(), dma=False, name=""):
        i = Ins()
        i.eng = eng
        i.fn = fn
        i.reads = [r for r in (a if isinstance(a, tuple) else self.region(a) for a in reads) if r is not None]
        i.writes = [r for r in (a if isinstance(a, tuple) else self.region(a) for a in writes) if r is not None]
        i.dma = dma
        i.idx = len(self.ins)
        i.deps = set()
        i.sig = False
        i.semv = None
        i.name = name
        self.ins.append(i)
        return i

    def barrier(self):
        self.add("sp", lambda: self.nc.sync.nop(), writes=[("BAR", 0, 1, 0, 1)], name="bar1")
        for e in ("pe", "act", "dve", "pool"):
            self.add(e, lambda: None, reads=[("BAR", 0, 1, 0, 1)], name="bar2")

    def mm(self, out, lhsT, rhs, start=True, stop=True):
        return self.add("pe", lambda: self.nc.tensor.matmul(out, lhsT, rhs, start=start, stop=stop),
                        reads=[lhsT, rhs] + ([] if start else [out]), writes=[out])

    def tr(self, out, in_, ident):
        return self.add("pe", lambda: self.nc.tensor.transpose(out, in_, ident), reads=[in_, ident], writes=[out])

    def act(self, out, in_, func, bias=None, scale=None, accum_out=None):
        kw = {}
        rd = [in_]
        if bias is not None:
            kw["bias"] = bias
            if not isinstance(bias, (int, float)):
                rd.append(bias)
        if scale is not None:
            kw["scale"] = scale
            if not isinstance(scale, (int, float)):
                rd.append(scale)
        wr = [out]
        if accum_out is not None:
            kw["accum_out"] = accum_out
            wr.append(accum_out)
        return self.add("act", lambda: self.nc.scalar.activation(out, in_, func, **kw), reads=rd, writes=wr)

    def _veng(self, eng):
        return self.nc.vector if eng == "dve" else self.nc.gpsimd

    def ts(self, out, in0, s1, s2, op0, op1=None, eng="dve", accum_out=None):
        rd = [in0] + [s for s in (s1, s2) if s is not None and not isinstance(s, (int, float))]
        kw = {}
        wr = [out]
        if accum_out is not None:
            kw["accum_out"] = accum_out
            wr.append(accum_out)
        if op1 is None:
            return self.add(eng, lambda: self._veng(eng).tensor_scalar(out, in0, s1, None, op0, **kw), reads=rd, writes=wr)
        return self.add(eng, lambda: self._veng(eng).tensor_scalar(out, in0, s1, s2, op0, op1, **kw), reads=rd, writes=wr)

    def tt(self, out, in0, in1, op, eng="dve"):
        return self.add(eng, lambda: self._veng(eng).tensor_tensor(out, in0, in1, op), reads=[in0, in1], writes=[out])

    def stt(self, out, in0, scalar, in1, op0, op1):
        rd = [in0, in1] + ([] if isinstance(scalar, (int, float)) else [scalar])
        return self.add("dve", lambda: self.nc.vector.scalar_tensor_tensor(out, in0, scalar, in1, op0, op1), reads=rd, writes=[out])

    def copy(self, out, in_, eng="dve"):
        if eng == "act":
            return self.add("act", lambda: self.nc.scalar.copy(out, in_), reads=[in_], writes=[out])
        return self.add(eng, lambda: self._veng(eng).tensor_copy(out, in_), reads=[in_], writes=[out])

    def memset(self, out, val, eng="dve"):
        return self.add(eng, lambda: self._veng(eng).memset(out, val), reads=[], writes=[out])

    def recip(self, out, in_):
        return self.add("dve", lambda: self.nc.vector.reciprocal(out, in_), reads=[in_], writes=[out])

    def dma(self, out, in_, eng="sp", rd=None, wr=None, **kw):
        return self.add(eng, lambda: self.engs[eng].dma_start(out, in_, **kw),
                        reads=[in_] if rd is None else rd, writes=[out] if wr is None else wr, dma=True)

    def emit(self):
        nc = self.nc
        recs = {}
        last_on = {}
        dmas_open = []

        def ovl(r, q):
            return r[0] < q[2] and q[1] < r[1] and r[2] < q[4] and q[3] < r[3]

        for ins in self.ins:
            deps = ins.deps
            if ins.name == "bar1":
                deps.update(last_on.values())
                deps.update(dmas_open)
                dmas_open = []
                recs = {}
            for q in ins.reads:
                isps = self.tinfo.get(q[0], (0, ""))[1] == "ps"
                for r in recs.get(q[0], ()):
                    if ovl(r, q):
                        if r[4] is not None:
                            deps.add(r[4])
                        if isps:
                            deps.update(j for e_, j in r[5].items() if e_ != ins.eng)
                        if ins.dma:
                            r[6].append(ins.idx)
                        else:
                            r[5][ins.eng] = ins.idx
            for q in ins.writes:
                lst = recs.get(q[0], ())
                keep = []
                for r in lst:
                    if ovl(r, q):
                        if r[4] is not None:
                            deps.add(r[4])
                        deps.update(r[5].values())
                        deps.update(r[6])
                        if q[1] <= r[0] and r[1] <= q[2] and q[3] <= r[2] and r[3] <= q[4]:
                            continue
                    keep.append(r)
                keep.append([q[1], q[2], q[3], q[4], ins.idx, {}, []])
                recs[q[0]] = keep
            deps.discard(ins.idx)
            if ins.dma:
                dmas_open.append(ins.idx)
            elif ins.name not in ("bar1", "bar2"):
                last_on[ins.eng] = ins.idx

        for ins in self.ins:
            nd = set()
            for j in ins.deps:
                p = self.ins[j]
                if p.eng == ins.eng and not p.dma and (ins.eng not in self.self_sync):
                    continue
                nd.add(j)
            ins.deps = nd
            for j in nd:
                self.ins[j].sig = True

        self._sems = []

        def newsem(nm):
            g = nc.semaphore(nm)
            s = g.__enter__()
            self._sems.append(g)
            return s

        eng_sems = {e: [] for e in self.engs}
        eng_cnt = {e: 0 for e in self.engs}
        dma_sems = [newsem(f"dq{k}") for k in range(DMA_POOL)]
        dma_n = 0
        known = {e: {} for e in self.engs}

        def wait(engname, sem, val):
            k = known[engname]
            key = id(sem)
            if k.get(key, 0) >= val:
                return
            k[key] = val
            self.engs[engname].wait_ge(sem, val)

        for ins in self.ins:
            e = ins.eng
            for j in sorted(ins.deps):
                p = self.ins[j]
                if p.semv is None:
                    raise RuntimeError(f"dep on unsignaled instr {p.name} {p.eng}")
                wait(e, p.semv[0], p.semv[1])
            if ins.dma:
                slot = dma_n % DMA_POOL
                val = 16 * (dma_n // DMA_POOL + 1)
                if val > 16:
                    wait(e, dma_sems[slot], val - 16)
                dma_n += 1
                ins.semv = (dma_sems[slot], val)
                ins.fn().then_inc(dma_sems[slot], 16)
            else:
                bi = ins.fn()
                if ins.sig:
                    if bi is None:
                        raise RuntimeError("signal needed on empty instr")
                    c = eng_cnt[e]
                    ph = c // SEM_PHASE
                    while len(eng_sems[e]) <= ph:
                        eng_sems[e].append(newsem(f"s_{e}{len(eng_sems[e])}"))
                    sem = eng_sems[e][ph]
                    eng_cnt[e] = c + 1
                    ins.semv = (sem, c % SEM_PHASE + 1)
                    bi.then_inc(sem, 1)
        self.n_dma = dma_n
        self.counts = dict(eng_cnt)


def make_consts():
    i = np.arange(128)
    ident = np.eye(128, dtype=np.float32)
    U = (i[:, None] <= i[None, :]).astype(np.float32)
    GT = (i[:, None] > i[None, :]).astype(np.float32)
    lv = []
    for l in range(7):
        b = 1 << l
        m = ((i[:, None] // (2 * b) == i[None, :] // (2 * b)) & ((i[:, None] // b) % 2 == 1)
             & ((i[None, :] // b) % 2 == 0))
        lv.append(-m.astype(np.float32))
    ones = np.ones((128, 128), np.float32)
    return np.concatenate([ident, U, GT] + lv + [ones], axis=1)


class Builder:
    def __init__(self, S, NE, debug=False, phases="0ABC"):
        self.S = S
        self.NE = NE
        self.NT = S // 128
        self.debug = debug
        self.phases = phases
        nc = bass.Bass("TRN2", target_bir_lowering=False)
        self.nc = nc
        self.P = Prog(nc)
        self.decl = set()
        self.build()

    def din(self, name, shape, dtype=F32, ph="0ABC"):
        if not any(p in self.phases for p in ph):
            return None
        self.decl.add(name)
        return self.nc.dram_tensor(name, list(shape), dtype, kind="ExternalInput").ap()

    def scratch(self, name, shape, dtype):
        kind = "ExternalOutput" if self.debug else "Internal"
        t = self.nc.dram_tensor(name, list(shape), dtype, kind=kind)
        self.P.tinfo[t.name] = (None, "dr")
        return t.ap()

    def build(self):
        P, nc, S, NE = self.P, self.nc, self.S, self.NE
        self.x = self.din("x", [S, D])
        self.c_in = self.din("c", [128, 16])
        self.w_ada = self.din("w_ada", [D, 6 * D], ph="0")
        self.b_ada = self.din("b_ada", [1, 6 * D], ph="0")
        self.w_in_m = self.din("w_in_m", [8, 128, 16 * 770], ph="A")
        self.w_in_g = self.din("w_in_g", [16, 128, 16 * 772], ph="A")
        self.w_rarb = self.din("w_rarb", [16, 128, 4096], ph="B")
        self.m_bias = self.din("m_bias", [1, 16], ph="A")
        self.m_norm_w = self.din("m_norm_w", [1, D], ph="A")
        self.conv_g = self.din("conv_g", [16, 128, 16], ph="A")
        self.g_ab = self.din("g_ab", [1, 64], ph="A")
        self.g_norm_w = self.din("g_norm_w", [1, 128], ph="A")
        self.w_a = self.din("w_a", [16, 128, 2048], ph="B")
        self.w_b = self.din("w_b", [16, 128, 4096], ph="B")
        self.w_out = self.din("w_out", [4, 128, 8192], ph="B")
        self.lnp = self.din("lnp", [4, D])
        self.w_router = self.din("w_router", [D, NE], ph="B")
        self.b_router = self.din("b_router", [1, NE], ph="B")
        self.EG = min(8, NE)
        self.w_ups = [self.din(f"w_up{i}", [self.EG, 16, 128, 4096], ph="C") for i in range(NE // self.EG)]
        self.b_upg = self.din("b_upg", [128, NE * 16], ph="C")
        self.b_upu = self.din("b_upu", [128, NE * 16], ph="C")
        self.w_downs = [self.din(f"w_down{i}", [self.EG, 8, 128, 4096], ph="C") for i in range(NE // self.EG)]
        self.b_down = self.din("b_down", [NE, D], ph="C")
        self.consts_in = self.din("consts", [128, NCONST])
        self.out = self.nc.dram_tensor("out", [S, D], F32, kind="ExternalOutput").ap()
        P.tinfo[self.out.tensor.name] = (None, "dr")

        self.modrow = self.scratch("modrow", [1, 6 * D], F32)
        self.hT = self.scratch("hT", [D, S], BF16)
        self.mix = self.scratch("mix", [S, 3 * D], BF16)
        self.pre = self.scratch("pre", [S, D], F32)
        self.x1 = self.scratch("x1", [S, D], F32)
        self.h2T = self.scratch("h2T", [D, S], BF16)
        self.gates_d = self.scratch("gates", [S, NE], F32)
        self.ffn = self.scratch("ffn", [S, D], F32)

        self.cst = P.sbuf("cst", [128, NCONST], F32)
        P.dma(self.cst[:, :], self.consts_in[:, :])
        c = self.cst
        self.ident = c[:, 0:128]
        self.U = c[:, 128:256]
        self.GT = c[:, 256:384]
        self.negm = [c[:, 384 + 128 * l: 512 + 128 * l] for l in range(7)]
        self.ones = c[:, 1280:1408]
        self.identb = P.sbuf("identb", [128, 128], BF16)
        P.copy(self.identb[:, :], self.ident)
        self.bi = 0
        self.qbi = 0
        self.pn = 0

        if "0" in self.phases:
            self.phase0()
        if "A" in self.phases:
            self.phaseA()
        if "B" in self.phases:
            self.phaseB()
        if "C" in self.phases:
            self.phaseC()
        P.add("sp", lambda: None, reads=[self.out[:, :]] if "C" in self.phases else [], writes=[], name="fin")
        P.barrier()
        P.emit()

    def set_psum(self, nf, nb):
        self.pn += 1
        self.big = [self.P.psum(f"pf{self.pn}_{i}", [128, 512], F32) for i in range(nf)]
        self.bfb = [self.P.psum(f"pb{self.pn}_{i}", [128, 1024], BF16) for i in range(nb)]

    def bank(self):
        b = self.big[self.bi % len(self.big)]
        self.bi += 1
        return b

    def q(self, n=128):
        return self.bank()[:, 0:n]

    def ln_stats(self, src, st, mv, rstd, nmr, eps):
        P, nc = self.P, self.nc
        for k in range(4):
            P.add("dve", (lambda k=k: nc.vector.bn_stats(st[:, k * 6:(k + 1) * 6], src[:, k * 512:(k + 1) * 512])),
                  reads=[src[:, k * 512:(k + 1) * 512]], writes=[st[:, k * 6:(k + 1) * 6]])
        P.add("dve", lambda: nc.vector.bn_aggr(mv[:, 0:2], st[:, 0:24]), reads=[st[:, 0:24]], writes=[mv[:, 0:2]])
        P.ts(mv[:, 2:3], mv[:, 1:2], eps, None, ALU.add)
        P.act(mv[:, 3:4], mv[:, 2:3], AF.Ln)
        P.act(rstd, mv[:, 3:4], AF.Exp, scale=-0.5)
        P.ts(nmr, mv[:, 0:1], -1.0, rstd, ALU.mult, ALU.mult)

    def rstd_from_ss(self, ss, n, tmp, rstd):
        P = self.P
        P.ts(tmp, ss, 1.0 / n, RMS_EPS, ALU.mult, ALU.add)
        P.act(tmp, tmp, AF.Ln)
        P.act(rstd, tmp, AF.Exp, scale=-0.5)

    def softplus_parts(self, y, a, e, l):
        P = self.P
        P.stt(a, y, -1.0, y, ALU.mult, ALU.max)
        P.act(e, a, AF.Exp, scale=-1.0)
        P.ts(e, e, 1.0, None, ALU.add)
        P.act(l, e, AF.Ln)

    def phase0(self):
        import os
        BIS = int(os.environ.get("BIS", "255"))
        P, nc, S = self.P, self.nc, self.S
        mk = P.mark()
        self.set_psum(2, 4)
        csb = P.sbuf("csb", [128, 16], F32)
        scs = P.sbuf("scs", [128, 16], F32)
        P.dma(csb[:, :], self.c_in[:, :])
        P.act(scs[:, :], csb[:, :], AF.Silu)
        wt = [P.sbuf(f"wada{i}", [128, 16, 512], F32) for i in range(2)]
        bt = [P.sbuf(f"bada{i}", [1, 512], F32) for i in range(2)]
        row = [P.sbuf(f"mrow{i}", [1, 512], F32) for i in range(2)]
        wv = self.w_ada.rearrange("(p c) n -> p c n", c=16)
        for n in range(24 if BIS & 1 else 0):
            w = wt[n % 2]
            P.dma(w[:, :, :], wv[:, :, n * 512:(n + 1) * 512])
            ps = self.bank()
            for cc in range(16):
                P.mm(ps[0:1, :], scs[:, cc:cc + 1], w[:, cc, :], start=(cc == 0), stop=(cc == 15))
            P.dma(bt[n % 2][:, :], self.b_ada[0:1, n * 512:(n + 1) * 512])
            P.tt(row[n % 2][0:1, :], ps[0:1, :], bt[n % 2][0:1, :], ALU.add)
            P.dma(self.modrow[0:1, n * 512:(n + 1) * 512], row[n % 2][:, :])
        sc1 = P.sbuf("sc1T", [128, 16], F32)
        sh1 = P.sbuf("sh1T", [128, 16], F32)
        if BIS & 2:
            P.dma(sh1[:, :], self.modrow[0, 0:D].rearrange("(c p) -> p c", p=128), allow_slow_non_contiguous=True)
            P.dma(sc1[:, :], self.modrow[0, D:2 * D].rearrange("(c p) -> p c", p=128), allow_slow_non_contiguous=True)
        else:
            P.memset(sh1[:, :], 0.0)
            P.memset(sc1[:, :], 0.0)
        P.ts(sc1[:, :], sc1[:, :], 1.0, None, ALU.add)
        xt = [P.sbuf(f"x0_{i}", [128, D], F32) for i in range(2)]
        xn = [P.sbuf(f"xn0_{i}", [128, D], BF16) for i in range(2)]
        ho = [P.sbuf(f"ho_{i}", [128, 16, 128], BF16) for i in range(2)]
        st = P.sbuf("st0", [128, 24], F32)
        mv = P.sbuf("mv0", [128, 8], F32)
        ptb = [P.psum(f"ptb{i}", [128, 4, 128], BF16) for i in range(2)] if False else None
        hTv = self.hT.rearrange("(c p) s -> p c s", p=128)
        for t in range(self.NT if BIS & 4 else 0):
            x_ = xt[t % 2]
            P.dma(x_[:, :], self.x[t * 128:(t + 1) * 128, :])
            self.ln_stats(x_, st, mv, mv[:, 4:5], mv[:, 5:6], LN_EPS)
            xn_ = xn[t % 2]
            P.act(xn_[:, :], x_[:, :], AF.Identity, bias=mv[:, 5:6], scale=mv[:, 4:5])
            h_ = ho[t % 2]
            for c4 in range(4):
                pb = self.qb()
                for k in range(4):
                    cc = c4 * 4 + k
                    P.tr(pb[:, k * 128:(k + 1) * 128], xn_[:, cc * 128:(cc + 1) * 128], self.identb[:, :])
                for k in range(4):
                    cc = c4 * 4 + k
                    P.ts(h_[:, cc, :], pb[:, k * 128:(k + 1) * 128], sc1[:, cc:cc + 1], sh1[:, cc:cc + 1], ALU.mult, ALU.add)
            if BIS & 16:
                P.dma(hTv[:, :, t * 128:(t + 1) * 128], h_[:, :, :], wr=[P.uniq("hT")])
        P.release(mk)

    def qb(self):
        b = self.bfb[self.qbi % len(self.bfb)]
        self.qbi += 1
        return b

    def phaseA(self):
        P, nc, S = self.P, self.nc, self.S
        mk = P.mark()
        self.set_psum(8, 0)
        NTL = S // 512
        hTv = self.hT.rearrange("(c p) s -> p c s", p=128)
        hbuf = [P.sbuf(f"hA{i}", [128, 16, 512], BF16) for i in range(2)]
        wbuf = [P.sbuf(f"wA{i}", [128, 16, 772], BF16) for i in range(2)]
        mb = P.sbuf("mb", [128, 16], F32)
        P.dma(mb[:, :], self.m_bias[0:1, :].partition_broadcast(128))
        P.ts(mb[:, 0:8], mb[:, 0:8], 2.0 / 15.0, None, ALU.mult)
        gab = P.sbuf("gab", [128, 64], F32)
        P.dma(gab[:, :], self.g_ab[0:1, :].partition_broadcast(128))
        P.act(gab[:, 0:32], gab[:, 0:32], AF.Exp)
        P.ts(gab[:, 0:32], gab[:, 0:32], -1.0, None, ALU.mult)
        gnw = P.sbuf("gnw", [128, 128], F32)
        P.dma(gnw[:, :], self.g_norm_w[0:1, :].partition_broadcast(128))
        mnw = P.sbuf("mnw", [128, 256], F32)
        cw = P.sbuf("cw", [128, 16], F32)

        W = {}

        def wt(name, shape=(128, 128), dt=F32, n=2):
            W[name] = [P.sbuf(f"A_{name}{i}", list(shape), dt) for i in range(n)]

        for nm in ["Fm", "fbc", "DmT", "DmTm", "eb", "SmT", "QdT", "kd", "ktm", "dec", "decT", "eGb", "decmb",
                   "decTm", "attnT", "Nm", "NT", "T", "R", "Y", "tmp", "vb", "kbg", "kdec", "nwT", "vnew", "sz", "t1"]:
            wt(nm)
        wt("v1", (128, 257))
        wt("tk3", (128, 260))
        wt("KK")
        wt("AT")
        wt("hraw", (128, 256))
        wt("sig", (128, 256))
        wt("junk", (128, 256))
        wt("hm", (128, 256), BF16)
        wt("on", (128, 128), BF16)
        wt("col", (128, 16), n=4)
        wt("gsh", (128, 16))
        self.wi = {k: 0 for k in W}

        def g(name):
            i = self.wi[name]
            self.wi[name] = i + 1
            return W[name][i % len(W[name])]

        qT_all = P.sbuf("qT_all", [128, 512], F32)
        kT_all = P.sbuf("kT_all", [128, 512], F32)
        Cn = P.sbuf("Cn", [128, 257], F32)
        for v in W["v1"]:
            P.memset(v[:, 256:257], 1.0)

        load_n = [0]

        def load_h(T):
            hb = hbuf[load_n[0] % 2]
            load_n[0] += 1
            P.dma(hb[:, :, :], hTv[:, :, T * 512:(T + 1) * 512], rd=[P.uniq("hTr")])
            return hb

        for hd in range(8):
            wm = wbuf[hd % 2]
            P.dma(wm[:, :, 0:770], self.w_in_m[hd].rearrange("p (c n) -> p c n", c=16), eng="pool")
            P.dma(mnw[:, :], self.m_norm_w[0:1, hd * 256:(hd + 1) * 256].partition_broadcast(128))
            P.memset(Cn[:, :], 0.0)
            hb_next = load_h(0)
            for T in range(NTL):
                hb = hb_next
                if T + 1 < NTL:
                    hb_next = load_h(T + 1)
                for ct, dst, sc in ((0, qT_all, 1.0), (1, kT_all, 128.0 ** -0.5)):
                    ps = self.bank()
                    for cc in range(16):
                        P.mm(ps[:, :], wm[:, cc, ct * 128:(ct + 1) * 128], hb[:, cc, :], start=(cc == 0), stop=(cc == 15))
                    P.act(dst[:, :], ps[:, :], AF.Copy, scale=sc)
                for j in range(4):
                    tok0 = T * 512 + j * 128
                    ps1 = self.bank()
                    ps2 = self.bank()
                    for cc in range(16):
                        P.mm(ps1[:, 0:386], hb[:, cc, j * 128:(j + 1) * 128], wm[:, cc, 128:514], start=(cc == 0), stop=(cc == 15))
                    for cc in range(16):
                        P.mm(ps2[:, 0:256], hb[:, cc, j * 128:(j + 1) * 128], wm[:, cc, 514:770], start=(cc == 0), stop=(cc == 15))
                    qT = qT_all[:, j * 128:(j + 1) * 128]
                    kT = kT_all[:, j * 128:(j + 1) * 128]
                    col = g("col")
                    ktm = g("ktm")
                    P.act(ktm[:, :], ps1[:, 0:128], AF.Copy, scale=128.0 ** -0.5)
                    v1 = g("v1")
                    P.copy(v1[:, 0:256], ps1[:, 128:384])
                    sig = g("sig")
                    P.act(sig[:, :], ps2[:, 0:256], AF.Exp, scale=-1.0)
                    P.ts(sig[:, :], sig[:, :], 1.0, None, ALU.add)
                    P.recip(sig[:, :], sig[:, :])
                    P.copy(col[:, 12:14], ps1[:, 384:386])
                    P.act(col[:, 0:1], col[:, 12:13], AF.Exp, bias=mb[:, hd:hd + 1], scale=2.0 / 15.0)
                    P.ts(col[:, 0:1], col[:, 0:1], 1.0, None, ALU.add)
                    P.recip(col[:, 0:1], col[:, 0:1])
                    P.ts(col[:, 1:2], col[:, 0:1], -30.0, 15.0, ALU.mult, ALU.add)
                    P.ts(col[:, 2:3], col[:, 13:14], mb[:, 8 + hd:9 + hd], None, ALU.add)
                    self.softplus_parts(col[:, 2:3], col[:, 3:4], col[:, 4:5], col[:, 5:6])
                    P.stt(col[:, 6:7], col[:, 2:3], 0.0, col[:, 5:6], ALU.min, ALU.subtract)
                    Fm = g("Fm")
                    fbc = g("fbc")
                    P.ts(Fm[:, :], self.GT, col[:, 6:7], None, ALU.mult)
                    P.ts(fbc[:, :], self.ones, col[:, 6:7], None, ALU.mult)
                    psD = self.q()
                    psE = self.q()
                    P.mm(psD, Fm[:, :], self.U)
                    P.mm(psE, fbc[:, :], self.U)
                    DmT = g("DmT")
                    P.act(DmT[:, :], psD, AF.Exp, bias=col[:, 1:2])
                    DmTm = g("DmTm")
                    P.tt(DmTm[:, :], DmT[:, :], self.U, ALU.mult)
                    eb = g("eb")
                    P.act(eb[:, :], psE, AF.Exp)
                    psS = self.q()
                    P.mm(psS, kT, qT)
                    SmT = g("SmT")
                    P.tt(SmT[:, :], psS, DmTm[:, :], ALU.mult)
                    QdT = g("QdT")
                    P.tt(QdT[:, :], qT, eb[:, :], ALU.mult)
                    psN = self.bank()
                    P.mm(psN[:, 0:257], QdT[:, :], Cn[:, :], start=True, stop=False)
                    P.mm(psN[:, 0:257], SmT[:, :], v1[:, :], start=False, stop=True)
                    kd = g("kd")
                    P.ts(kd[:, :], ktm[:, :], DmT[:, 127:128], None, ALU.mult)
                    psC = self.bank()
                    P.mm(psC[:, 0:257], kd[:, :], v1[:, :])
                    P.stt(Cn[:, :], Cn[:, :], eb[:, 127:128], psC[:, 0:257], ALU.mult, ALU.add)
                    P.copy(col[:, 14:15], psN[:, 256:257])
                    P.stt(col[:, 7:8], col[:, 14:15], -1.0, col[:, 14:15], ALU.mult, ALU.max)
                    P.ts(col[:, 7:8], col[:, 7:8], 1.0, None, ALU.max)
                    P.recip(col[:, 8:9], col[:, 7:8])
                    hraw = g("hraw")
                    P.ts(hraw[:, :], psN[:, 0:256], col[:, 8:9], None, ALU.mult)
                    junk = g("junk")
                    P.act(junk[:, :], hraw[:, :], AF.Square, accum_out=col[:, 9:10])
                    self.rstd_from_ss(col[:, 9:10], 256.0, col[:, 10:11], col[:, 11:12])
                    t1 = g("junk")
                    P.stt(t1[:, :], hraw[:, :], col[:, 11:12], mnw[:, :], ALU.mult, ALU.mult)
                    hm = g("hm")
                    P.tt(hm[:, :], t1[:, :], sig[:, :], ALU.mult)
                    P.dma(self.mix[tok0:tok0 + 128, hd * 256:(hd + 1) * 256], hm[:, :], wr=[P.uniq("mix")])

        xc = P.sbuf("xc", [128, 4, 515], F32)
        cv = P.sbuf("cv", [128, 4, 512], F32)
        cs = P.sbuf("cs", [128, 4, 512], F32)
        sq = P.sbuf("sq", [128, 2, 512], F32)
        rn = P.sbuf("rn", [128, 2, 512], F32)
        Sst = [P.sbuf(f"Sst{i}", [128, 128], F32) for i in range(2)]
        for jh in range(16):
            wg = wbuf[jh % 2]
            P.dma(wg[:, :, :].rearrange("p c n -> p (c n)"), self.w_in_g[jh], eng="pool", max_dma_last_dim=8192)
            P.dma(cw[:, :], self.conv_g[jh])
            P.memset(xc[:, :, 0:3], 0.0)
            for s_ in Sst:
                P.memset(s_[:, :], 0.0)
            hb_next = load_h(0)
            for T in range(NTL):
                hb = hb_next
                if T + 1 < NTL:
                    hb_next = load_h(T + 1)
                for ct in range(4):
                    ps = self.bank()
                    for cc in range(16):
                        P.mm(ps[:, :], wg[:, cc, ct * 128:(ct + 1) * 128], hb[:, cc, :], start=(cc == 0), stop=(cc == 15))
                    P.copy(xc[:, ct, 3:515], ps[:, :], eng="act")
                for ct in range(4):
                    P.ts(cv[:, ct, :], xc[:, ct, 0:512], cw[:, ct * 4:ct * 4 + 1], None, ALU.mult)
                    for k in range(1, 4):
                        P.stt(cv[:, ct, :], xc[:, ct, k:k + 512], cw[:, ct * 4 + k:ct * 4 + k + 1], cv[:, ct, :], ALU.mult, ALU.add)
                P.copy(xc[:, :, 0:3], xc[:, :, 512:515])
                P.act(cs[:, :, :], cv[:, :, :], AF.Silu)
                P.act(sq[:, :, :], cs[:, 0:2, :], AF.Square)
                for ct in range(2):
                    ps = self.bank()
                    P.mm(ps[:, :], self.ones, sq[:, ct, :])
                    P.ts(rn[:, ct, :], ps[:, :], RMS_EPS, None, ALU.add)
                P.act(rn[:, :, :], rn[:, :, :], AF.Ln)
                P.act(rn[:, :, :], rn[:, :, :], AF.Exp, scale=-0.5)
                P.stt(qT_all[:, :], cs[:, 0, :], 128.0 ** -0.5, rn[:, 0, :], ALU.mult, ALU.mult)
                P.tt(kT_all[:, :], cs[:, 1, :], rn[:, 1, :], ALU.mult)
                for j in range(4):
                    tok0 = T * 512 + j * 128
                    sl = slice(j * 128, (j + 1) * 128)
                    ps3 = self.bank()
                    for cc in range(16):
                        P.mm(ps3[:, 0:260], hb[:, cc, sl], wg[:, cc, 512:772], start=(cc == 0), stop=(cc == 15))
                    qT = qT_all[:, sl]
                    kT = kT_all[:, sl]
                    psK = self.q()
                    P.tr(psK, kT, self.ident)
                    ktm = g("ktm")
                    P.copy(ktm[:, :], psK, eng="act")
                    tk3 = g("tk3")
                    P.copy(tk3[:, :], ps3[:, 0:260], eng="act")
                    ps3 = tk3
                    psKK_ = self.q()
                    P.mm(psKK_, kT, kT)
                    psKK = g("KK")
                    P.copy(psKK[:, :], psKK_, eng="act")
                    psKK = psKK[:, :]
                    psAT_ = self.q()
                    P.mm(psAT_, kT, qT)
                    psAT = g("AT")
                    P.copy(psAT[:, :], psAT_)
                    psAT = psAT[:, :]
                    gsh = g("gsh")
                    P.act(gsh[:, 0:2], ps3[:, 2:4], AF.Exp, scale=-1.0)
                    P.ts(gsh[:, 0:2], gsh[:, 0:2], 1.0, None, ALU.add)
                    P.recip(gsh[:, 0:2], gsh[:, 0:2])
                    P.tt(gsh[:, 2:4], ps3[:, 0:2], gab[:, 32 + 2 * jh:34 + 2 * jh], ALU.add)
                    self.softplus_parts(gsh[:, 2:4], gsh[:, 4:6], gsh[:, 6:8], gsh[:, 8:10])
                    P.stt(gsh[:, 10:12], gsh[:, 2:4], 0.0, gsh[:, 8:10], ALU.max, ALU.add)
                    P.tt(gsh[:, 12:14], gsh[:, 10:12], gab[:, 2 * jh:2 * jh + 2], ALU.mult)
                    def chain(hv, gsh=gsh, ps3=ps3, psKK=psKK, psAT=psAT, qT=qT, kT=kT, ktm=ktm, tok0=tok0, sl=sl):
                        hh = 2 * jh + hv
                        col = g("col")
                        yield
                        Fm = g("Fm")
                        gbc = g("fbc")
                        P.ts(Fm[:, :], self.GT, gsh[:, 12 + hv:13 + hv], None, ALU.mult)
                        P.ts(gbc[:, :], self.ones, gsh[:, 12 + hv:13 + hv], None, ALU.mult)
                        psD = self.q()
                        psDT = self.q()
                        psEG = self.q()
                        psG = self.q()
                        P.mm(psD, self.U, Fm[:, :])
                        P.mm(psDT, Fm[:, :], self.U)
                        P.mm(psEG, gbc[:, :], self.U)
                        P.mm(psG[:, 0:1], self.U, gsh[:, 12 + hv:13 + hv])
                        dec = g("dec")
                        decT = g("decT")
                        eGb = g("eGb")
                        P.act(dec[:, :], psD, AF.Exp)
                        P.act(decT[:, :], psDT, AF.Exp)
                        P.act(eGb[:, :], psEG, AF.Exp)
                        P.act(col[:, 7:8], psG[:, 0:1], AF.Exp)
                        decmb = g("decmb")
                        P.stt(decmb[:, :], dec[:, :], gsh[:, hv:hv + 1], self.GT, ALU.mult, ALU.mult)
                        Nm = g("Nm")
                        P.tt(Nm[:, :], psKK, decmb[:, :], ALU.mult)
                        decTm = g("decTm")
                        P.tt(decTm[:, :], decT[:, :], self.U, ALU.mult)
                        attnT = g("attnT")
                        P.tt(attnT[:, :], psAT, decTm[:, :], ALU.mult)
                        QdT = g("QdT")
                        P.tt(QdT[:, :], qT, eGb[:, :], ALU.mult)
                        yield
                        psVt = self.q()
                        P.tr(psVt, cs[:, 2 + hv, sl], self.ident)
                        vb = g("vb")
                        P.ts(vb[:, :], psVt, gsh[:, hv:hv + 1], None, ALU.mult)
                        P.tt(col[:, 8:9], gsh[:, hv:hv + 1], col[:, 7:8], ALU.mult)
                        kbg = g("kbg")
                        P.ts(kbg[:, :], ktm[:, :], col[:, 8:9], None, ALU.mult)
                        kdec = g("kdec")
                        P.ts(kdec[:, :], ktm[:, :], decT[:, 127:128], None, ALU.mult)
                        yield
                        psNT = self.q()
                        P.tr(psNT, Nm[:, :], self.ident)
                        NT_ = g("NT")
                        P.copy(NT_[:, :], psNT, eng="act")
                        tmp = g("tmp")
                        P.tt(tmp[:, :], Nm[:, :], self.negm[0], ALU.mult)
                        Tc = g("T")
                        P.tt(Tc[:, :], tmp[:, :], self.ident, ALU.add)
                        psR = self.q()
                        P.tr(psR, Tc[:, :], self.ident)
                        Rc = g("R")
                        P.copy(Rc[:, :], psR, eng="act")
                        yield
                        for l in range(1, 7):
                            psY = self.q()
                            P.mm(psY, NT_[:, :], Tc[:, :])
                            Y = g("Y")
                            P.copy(Y[:, :], psY, eng="act")
                            yield
                            psZ = self.q()
                            P.mm(psZ, Rc[:, :], Y[:, :])
                            tmp = g("tmp")
                            P.tt(tmp[:, :], psZ, self.negm[l], ALU.mult)
                            Tn = g("T")
                            P.tt(Tn[:, :], tmp[:, :], Tc[:, :], ALU.add)
                            Tc = Tn
                            yield
                            psR = self.q()
                            P.tr(psR, Tc[:, :], self.ident)
                            Rc = g("R")
                            P.copy(Rc[:, :], psR, eng="act")
                            yield
                        psW = self.q()
                        P.mm(psW, kbg[:, :], Rc[:, :])
                        nwT = g("nwT")
                        P.act(nwT[:, :], psW, AF.Copy, scale=-1.0)
                        yield
                        Sh = Sst[hv]
                        psV = self.q()
                        P.mm(psV, Rc[:, :], vb[:, :], start=True, stop=False)
                        P.mm(psV, nwT[:, :], Sh[:, :], start=False, stop=True)
                        vnew = g("vnew")
                        P.copy(vnew[:, :], psV, eng="act")
                        yield
                        psO = self.q()
                        P.mm(psO, QdT[:, :], Sh[:, :], start=True, stop=False)
                        P.mm(psO, attnT[:, :], vnew[:, :], start=False, stop=True)
                        psS = self.q()
                        P.mm(psS, kdec[:, :], vnew[:, :])
                        P.stt(Sh[:, :], Sh[:, :], eGb[:, 127:128], psS, ALU.mult, ALU.add)
                        yield
                        junk = g("tmp")
                        P.act(junk[:, :], psO, AF.Square, accum_out=col[:, 9:10])
                        self.rstd_from_ss(col[:, 9:10], 128.0, col[:, 10:11], col[:, 11:12])
                        sz = g("sz")
                        P.act(sz[:, :], ps3[:, 4 + hv * 128:4 + (hv + 1) * 128], AF.Exp, scale=-1.0)
                        P.ts(sz[:, :], sz[:, :], 1.0, None, ALU.add)
                        P.recip(sz[:, :], sz[:, :])
                        P.tt(sz[:, :], sz[:, :], ps3[:, 4 + hv * 128:4 + (hv + 1) * 128], ALU.mult)
                        t1 = g("t1")
                        P.stt(t1[:, :], psO, col[:, 11:12], gnw[:, :], ALU.mult, ALU.mult)
                        on = g("on")
                        P.tt(on[:, :], t1[:, :], sz[:, :], ALU.mult)
                        P.dma(self.mix[tok0:tok0 + 128, D + hh * 128:D + (hh + 1) * 128], on[:, :], wr=[P.uniq("mix")])
                    gens = [chain(0), chain(1)]
                    while gens:
                        for gn in list(gens):
                            try:
                                next(gn)
                            except StopIteration:
                                gens.remove(gn)
        P.release(mk)

    def phaseB(self):
        P, nc, S, NE = self.P, self.nc, self.S, self.NE
        TB = 256
        mk = P.mark()
        self.set_psum(5, 3)
        hTv = self.hT.rearrange("(c p) s -> p c s", p=128)
        mixtm = P.sbuf("mixtm", [128, 2, 3 * D], BF16)
        mixT = P.sbuf("mixT", [128, 48, TB], BF16)
        hTt = P.sbuf("hTB", [128, 16, TB], BF16)
        xt = P.sbuf("xB", [128, 2, D], F32)
        wa = [P.sbuf(f"waB{i}", [128, 16, 128], BF16) for i in range(2)]
        wb = [P.sbuf(f"wbB{i}", [128, 32, 128], BF16) for i in range(2)]
        wr = [P.sbuf(f"wrB{i}", [128, 16, 256], BF16) for i in range(2)]
        wo = [P.sbuf(f"woB{i}", [128, 16, 512], BF16) for i in range(2)]
        mT = P.sbuf("mergedT", [128, 16, TB], BF16)
        pre = P.sbuf("preB", [128, 2, D], F32)
        g1b = P.sbuf("g1b", [128, D], F32)
        sa = [P.sbuf(f"saB{i}", [128, TB], F32) for i in range(2)]
        sb_ = [P.sbuf(f"sbB{i}", [128, TB], F32) for i in range(2)]
        P.dma(g1b[:, :], self.modrow[0:1, 2 * D:3 * D].partition_broadcast(128))
        wn = 0
        for tb in range(S // TB):
            t0 = tb * TB
            P.dma(mixtm[:, :, :], self.mix[t0:t0 + TB, :].rearrange("(s p) f -> p s f", p=128), rd=[P.uniq("mixr")])
            P.dma(hTt[:, :, :], hTv[:, :, t0:t0 + TB], rd=[P.uniq("hTr")])
            P.dma(xt[:, :, :], self.x[t0:t0 + TB, :].rearrange("(s p) f -> p s f", p=128))
            k = 0
            for s in range(2):
                for f4 in range(12):
                    pb = self.qb()
                    for kk in range(4):
                        fc = f4 * 4 + kk
                        P.tr(pb[:, kk * 128:(kk + 1) * 128], mixtm[:, s, fc * 128:(fc + 1) * 128], self.identb[:, :])
                    P.copy(mixT[:, f4 * 4:f4 * 4 + 4, s * 128:(s + 1) * 128], pb[:, 0:512].rearrange("p (k t) -> p k t", k=4),
                           eng=("act" if k % 2 else "dve"))
                    k += 1
            for n in range(16):
                a_, b_, r_ = wa[wn % 2], wb[wn % 2], wr[wn % 2]
                wn += 1
                P.dma(a_[:, :, :].rearrange("p c n -> p (c n)"), self.w_a[n], eng="pool", max_dma_last_dim=8192)
                P.dma(b_[:, :, :].rearrange("p c n -> p (c n)"), self.w_b[n], eng="pool", max_dma_last_dim=8192)
                P.dma(r_[:, :, :].rearrange("p c n -> p (c n)"), self.w_rarb[n], eng="pool", max_dma_last_dim=8192)
                pA, pB, pRa, pRb = self.bank(), self.bank(), self.bank(), self.bank()
                for cc in range(16):
                    P.mm(pA[:, 0:TB], a_[:, cc, :], mixT[:, cc, :], start=(cc == 0), stop=(cc == 15))
                for cc in range(32):
                    P.mm(pB[:, 0:TB], b_[:, cc, :], mixT[:, 16 + cc, :], start=(cc == 0), stop=(cc == 31))
                for cc in range(16):
                    P.mm(pRa[:, 0:TB], r_[:, cc, 0:128], hTt[:, cc, :], start=(cc == 0), stop=(cc == 15))
                for cc in range(16):
                    P.mm(pRb[:, 0:TB], r_[:, cc, 128:256], hTt[:, cc, :], start=(cc == 0), stop=(cc == 15))
                s1, s2 = sa[n % 2], sb_[n % 2]
                P.act(s1[:, :], pRa[:, 0:TB], AF.Sigmoid)
                P.act(s2[:, :], pRb[:, 0:TB], AF.Sigmoid)
                P.tt(s1[:, :], pA[:, 0:TB], s1[:, :], ALU.mult)
                P.tt(s2[:, :], pB[:, 0:TB], s2[:, :], ALU.mult)
                P.tt(mT[:, n, :], s1[:, :], s2[:, :], ALU.add)
            for cb in range(4):
                o_ = wo[cb % 2]
                P.dma(o_[:, :, :].rearrange("p c n -> p (c n)"), self.w_out[cb], eng="pool", max_dma_last_dim=8192)
                for s in range(2):
                    pM = self.bank()
                    for cc in range(16):
                        P.mm(pM[:, :], mT[:, cc, s * 128:(s + 1) * 128], o_[:, cc, :], start=(cc == 0), stop=(cc == 15))
                    dst = pre[:, s, cb * 512:(cb + 1) * 512]
                    P.tt(dst, pM[:, :], g1b[:, cb * 512:(cb + 1) * 512], ALU.mult)
                    P.stt(dst, xt[:, s, cb * 512:(cb + 1) * 512], ALPHA, dst, ALU.mult, ALU.add)
            P.dma(self.pre[t0:t0 + TB, :].rearrange("(s p) f -> p s f", p=128), pre[:, :, :], wr=[P.uniq("prew")])
        P.release(mk)

        mk = P.mark()
        self.set_psum(8, 0)
        lnb = [P.sbuf(f"lnb{i}", [128, D], F32) for i in range(4)]
        P.dma(lnb[0][:, :], self.lnp[0:1, :].partition_broadcast(128))
        P.dma(lnb[1][:, :], self.lnp[1:2, :].partition_broadcast(128))
        P.dma(lnb[3][:, :], self.modrow[0:1, 3 * D:4 * D].partition_broadcast(128))
        P.dma(lnb[2][:, :], self.modrow[0:1, 4 * D:5 * D].partition_broadcast(128))
        P.ts(lnb[2][:, :], lnb[2][:, :], 1.0, None, ALU.add)
        wrt = P.sbuf("wrt", [128, 16, NE], F32)
        P.dma(wrt[:, :, :], self.w_router.rearrange("(c p) n -> p c n", p=128))
        brb = P.sbuf("brb", [128, NE], F32)
        P.dma(brb[:, :], self.b_router[0:1, :].partition_broadcast(128))
        pt_ = [P.sbuf(f"pre2_{i}", [128, D], F32) for i in range(2)]
        x1t = [P.sbuf(f"x1t_{i}", [128, D], F32) for i in range(2)]
        h2t = [P.sbuf(f"h2t_{i}", [128, D], F32) for i in range(2)]
        h2Tf = [P.sbuf(f"h2Tf_{i}", [128, 16, 128], F32) for i in range(2)]
        h2Tb = [P.sbuf(f"h2Tb_{i}", [128, 16, 128], BF16) for i in range(2)]
        st = P.sbuf("stB", [128, 24], F32)
        mv = P.sbuf("mvB", [128, 8], F32)
        lg = [P.sbuf(f"lg{i}", [128, 3, NE], F32) for i in range(2)]
        m8 = P.sbuf("m8", [128, 16], F32)
        h2Tv = self.h2T.rearrange("(c p) s -> p c s", p=128)
        for t in range(self.NT):
            t0 = t * 128
            p_, x_, h_ = pt_[t % 2], x1t[t % 2], h2t[t % 2]
            P.dma(p_[:, :], self.pre[t0:t0 + 128, :], rd=[P.uniq("prer")])
            self.ln_stats(p_, st, mv, mv[:, 4:5], mv[:, 5:6], LN_EPS)
            P.act(x_[:, :], p_[:, :], AF.Identity, bias=mv[:, 5:6], scale=mv[:, 4:5])
            P.tt(x_[:, :], x_[:, :], lnb[0][:, :], ALU.mult)
            P.tt(x_[:, :], x_[:, :], lnb[1][:, :], ALU.add)
            P.dma(self.x1[t0:t0 + 128, :], x_[:, :], wr=[P.uniq("x1w")])
            self.ln_stats(x_, st, mv, mv[:, 4:5], mv[:, 5:6], LN_EPS)
            P.act(h_[:, :], x_[:, :], AF.Identity, bias=mv[:, 5:6], scale=mv[:, 4:5])
            P.tt(h_[:, :], h_[:, :], lnb[2][:, :], ALU.mult)
            P.tt(h_[:, :], h_[:, :], lnb[3][:, :], ALU.add)
            hf, hb = h2Tf[t % 2], h2Tb[t % 2]
            for cc in range(16):
                pq = self.q()
                P.tr(pq, h_[:, cc * 128:(cc + 1) * 128], self.ident)
                P.copy(hf[:, cc, :], pq, eng="act")
                P.copy(hb[:, cc, :], pq, eng="dve")
            P.dma(h2Tv[:, :, t0:t0 + 128], hb[:, :, :], wr=[P.uniq("h2Tw")])
            pl = self.q()
            for cc in range(16):
                P.mm(pl[:, 0:NE], hf[:, cc, :], wrt[:, cc, :], start=(cc == 0), stop=(cc == 15))
            l_ = lg[t % 2]
            P.tt(l_[:, 0, :], pl[:, 0:NE], brb[:, :], ALU.add)
            P.add("dve", (lambda l_=l_: nc.vector.max(m8[:, 0:8], l_[:, 0, :])), reads=[l_[:, 0, :]], writes=[m8[:, 0:8]])
            P.ts(l_[:, 1, :], l_[:, 0, :], m8[:, 3:4], None, ALU.is_ge)
            P.ts(m8[:, 8:9], m8[:, 0:1], -1.0, None, ALU.mult)
            P.act(l_[:, 2, :], l_[:, 0, :], AF.Exp, bias=m8[:, 8:9])
            P.tt(l_[:, 2, :], l_[:, 2, :], l_[:, 1, :], ALU.mult)
            P.add("dve", (lambda l_=l_: nc.vector.reduce_sum(m8[:, 9:10], l_[:, 2, :], mybir.AxisListType.X)),
                  reads=[l_[:, 2, :]], writes=[m8[:, 9:10]])
            P.recip(m8[:, 10:11], m8[:, 9:10])
            P.ts(l_[:, 0, :], l_[:, 2, :], m8[:, 10:11], None, ALU.mult)
            P.dma(self.gates_d[t0:t0 + 128, :], l_[:, 0, :], wr=[P.uniq("gw")])
        P.release(mk)

    def phaseC(self):
        P, nc, S, NE = self.P, self.nc, self.S, self.NE
        TG = min(1024, S)
        NS = TG // 128
        NH = TG // 512
        mk = P.mark()
        self.set_psum(8, 0)
        h2Tv = self.h2T.rearrange("(c p) s -> p c s", p=128)
        acc = P.sbuf("acc", [128, NS, D], F32)
        h2g = P.sbuf("h2g", [128, 16, TG], BF16)
        actT = P.sbuf("actT", [128, 16, TG], BF16)
        wu = [P.sbuf(f"wu{i}", [128, 16, 256], BF16) for i in range(2)]
        wd = [P.sbuf(f"wd{i}", [128, 16, 256], BF16) for i in range(2)]
        gt = P.sbuf("gt", [128, NS, NE], F32)
        gTs = P.sbuf("gTs", [NE, 128], F32)
        bd = P.sbuf("bd", [NE, D], F32)
        bg = P.sbuf("bg", [128, NE * 16], F32)
        bu = P.sbuf("bu", [128, NE * 16], F32)
        P.dma(bd[:, :], self.b_down[:, :])
        P.dma(bg[:, :], self.b_upg[:, :])
        P.dma(bu[:, :], self.b_upu[:, :])
        ew = [[P.sbuf(f"ew{k}_{i}", [128, 512], F32) for i in range(2)] for k in range(3)]
        wun = 0
        wdn = 0
        en = 0
        for G in range(S // TG):
            g0 = G * TG
            P.dma(h2g[:, :, :], h2Tv[:, :, g0:g0 + TG], rd=[P.uniq("h2Tr")])
            P.dma(gt[:, :, :], self.gates_d[g0:g0 + TG, :].rearrange("(s p) e -> p s e", p=128), rd=[P.uniq("gr")])
            for s in range(NS):
                pq = self.q()
                P.tr(pq[0:NE, :], gt[:, s, :], self.ident)
                P.copy(gTs[:, :], pq[0:NE, :], eng="act")
                for cb in range(4):
                    pb = self.bank()
                    P.mm(pb[:, :], gTs[:, :], bd[:, cb * 512:(cb + 1) * 512])
                    P.copy(acc[:, s, cb * 512:(cb + 1) * 512], pb[:, :], eng=("act" if cb % 2 else "dve"))
            for e in range(NE):
                for jt in range(16):
                    w_ = wu[wun % 2]
                    wun += 1
                    P.dma(w_[:, :, :].rearrange("p c n -> p (c n)"), self.w_ups[e // self.EG][e % self.EG, jt], eng="pool", max_dma_last_dim=8192)
                    for hf in range(NH):
                        tsl = slice(hf * 512, (hf + 1) * 512)
                        pG, pU = self.bank(), self.bank()
                        for cc in range(16):
                            P.mm(pG[:, :], w_[:, cc, 0:128], h2g[:, cc, tsl], start=(cc == 0), stop=(cc == 15))
                        for cc in range(16):
                            P.mm(pU[:, :], w_[:, cc, 128:256], h2g[:, cc, tsl], start=(cc == 0), stop=(cc == 15))
                        g_, s_, u_ = ew[0][en % 2], ew[1][en % 2], ew[2][en % 2]
                        en += 1
                        bi = e * 16 + jt
                        P.ts(g_[:, :], pG[:, :], bg[:, bi:bi + 1], 7.0, ALU.add, ALU.min)
                        P.act(s_[:, :], g_[:, :], AF.Sigmoid, scale=1.702)
                        P.ts(u_[:, :], pU[:, :], bu[:, bi:bi + 1], 7.0, ALU.add, ALU.min)
                        P.ts(u_[:, :], u_[:, :], -7.0, 1.0, ALU.max, ALU.add)
                        P.tt(g_[:, :], g_[:, :], s_[:, :], ALU.mult)
                        P.tt(actT[:, jt, tsl], g_[:, :], u_[:, :], ALU.mult)
                for cb in range(8):
                    d_ = wd[wdn % 2]
                    wdn += 1
                    P.dma(d_[:, :, :].rearrange("p c n -> p (c n)"), self.w_downs[e // self.EG][e % self.EG, cb], eng="pool", max_dma_last_dim=8192)
                    for s in range(NS):
                        pb = self.bank()
                        for cc in range(16):
                            P.mm(pb[:, 0:256], actT[:, cc, s * 128:(s + 1) * 128], d_[:, cc, :], start=(cc == 0), stop=(cc == 15))
                        dst = acc[:, s, cb * 256:(cb + 1) * 256]
                        P.stt(dst, pb[:, 0:256], gt[:, s, e:e + 1], dst, ALU.mult, ALU.add)
            P.dma(self.ffn[g0:g0 + TG, :].rearrange("(s p) f -> p s f", p=128), acc[:, :, :], wr=[P.uniq("ffnw")])
        P.release(mk)
        mk = P.mark()
        fb = [P.sbuf(f"fb{i}", [128, D], F32) for i in range(3)]
        P.dma(fb[0][:, :], self.modrow[0:1, 5 * D:6 * D].partition_broadcast(128))
        P.dma(fb[1][:, :], self.lnp[2:3, :].partition_broadcast(128))
        P.dma(fb[2][:, :], self.lnp[3:4, :].partition_broadcast(128))
        st = P.sbuf("stD", [128, 24], F32)
        mv = P.sbuf("mvD", [128, 8], F32)
        fa = [P.sbuf(f"fa{i}", [128, D], F32) for i in range(2)]
        x1b = [P.sbuf(f"x1b{i}", [128, D], F32) for i in range(2)]
        for t in range(self.NT):
            t0 = t * 128
            a_, x_ = fa[t % 2], x1b[t % 2]
            P.dma(a_[:, :], self.ffn[t0:t0 + 128, :], rd=[P.uniq("ffnr")])
            P.dma(x_[:, :], self.x1[t0:t0 + 128, :], rd=[P.uniq("x1r")])
            P.tt(a_[:, :], a_[:, :], fb[0][:, :], ALU.mult)
            P.stt(a_[:, :], x_[:, :], ALPHA, a_[:, :], ALU.mult, ALU.add)
            self.ln_stats(a_, st, mv, mv[:, 4:5], mv[:, 5:6], LN_EPS)
            P.act(x_[:, :], a_[:, :], AF.Identity, bias=mv[:, 5:6], scale=mv[:, 4:5])
            P.tt(x_[:, :], x_[:, :], fb[1][:, :], ALU.mult)
            P.tt(x_[:, :], x_[:, :], fb[2][:, :], ALU.add)
            P.dma(self.out[t0:t0 + 128, :], x_[:, :])
        P.release(mk)


def _prep_inputs(inputs, b, S, NE, shared=None):
    f = lambda a: np.ascontiguousarray(a, dtype=np.float32)
    if shared is not None:
        d = dict(shared)
        d["x"] = f(inputs["x"][b][:S])
        d["c"] = f(inputs["c"][b].reshape(128, 16))
        return d
    w_in = inputs["w_in"][0]
    wm = []
    for hd in range(8):
        cols = np.r_[hd * 128:(hd + 1) * 128, 1024 + hd * 128:1024 + (hd + 1) * 128,
                     2048 + hd * 256:2048 + (hd + 1) * 256, 4096 + hd, 4104 + hd,
                     4112 + hd * 256:4112 + (hd + 1) * 256]
        wm.append(w_in[:, cols])
    wg = []
    G0 = 6160
    for j in range(16):
        cols = np.r_[G0 + j * 128:G0 + (j + 1) * 128, G0 + 2048 + j * 128:G0 + 2048 + (j + 1) * 128,
                     G0 + 4096 + 2 * j * 128:G0 + 4096 + (2 * j + 2) * 128,
                     14352 + 2 * j:14352 + 2 * j + 2, 14384 + 2 * j:14384 + 2 * j + 2,
                     14416 + 2 * j * 128:14416 + (2 * j + 2) * 128]
        wg.append(w_in[:, cols])
    wrr = [np.concatenate([w_in[:, 18512 + n * 128:18512 + (n + 1) * 128],
                           w_in[:, 20560 + n * 128:20560 + (n + 1) * 128]], axis=1) for n in range(16)]
    conv = inputs["conv_w"][0]
    cg = []
    for j in range(16):
        tiles = [conv[:, j * 128:(j + 1) * 128], conv[:, 2048 + j * 128:2048 + (j + 1) * 128],
                 conv[:, 4096 + 2 * j * 128:4096 + (2 * j + 1) * 128], conv[:, 4096 + (2 * j + 1) * 128:4096 + (2 * j + 2) * 128]]
        cg.append(np.concatenate([t.T for t in tiles], axis=1))
    w_up = inputs["w_up"][0][:NE]
    w_up_r = w_up.reshape(NE, 16, 128, 16, 128, 2).transpose(0, 3, 2, 1, 5, 4).reshape(NE, 16, 128, 4096)
    w_down_r = inputs["w_down"][0][:NE].reshape(NE, 16, 128, 8, 256).transpose(0, 3, 2, 1, 4).reshape(NE, 8, 128, 4096)

    def pc(a, ncol):
        kc = a.shape[0] // 128
        return a.reshape(kc, 128, ncol).transpose(1, 0, 2).reshape(128, kc * ncol)
    b_up = inputs["b_up"][0][:NE].reshape(NE, 16, 128, 2)
    EG = min(8, NE)
    extra = {}
    for i in range(NE // EG):
        extra[f"w_up{i}"] = f(w_up_r[i * EG:(i + 1) * EG])
        extra[f"w_down{i}"] = f(w_down_r[i * EG:(i + 1) * EG])
    return {
        **extra,
        "x": f(inputs["x"][b][:S]),
        "c": f(inputs["c"][b].reshape(128, 16)),
        "w_ada": f(inputs["w_ada"][0]),
        "b_ada": f(inputs["b_ada"][0][None, :]),
        "w_in_m": f(np.stack([pc(a, 770) for a in wm])),
        "w_in_g": f(np.stack([pc(a, 772) for a in wg])),
        "w_rarb": f(np.stack([pc(a, 256) for a in wrr])),
        "m_bias": f(np.concatenate([inputs["m_bias_i"][0], inputs["m_bias_f"][0]])[None, :]),
        "m_norm_w": f(inputs["m_norm_w"][0][None, :]),
        "conv_g": f(np.stack(cg)),
        "g_ab": f(np.concatenate([inputs["g_a_log"][0], inputs["g_dt_bias"][0]])[None, :]),
        "g_norm_w": f(inputs["g_norm_w"][0][None, :]),
        "w_a": f(np.stack([pc(inputs["w_branch_a"][0][:, n * 128:(n + 1) * 128], 128) for n in range(16)])),
        "w_b": f(np.stack([pc(inputs["w_branch_b"][0][:, n * 128:(n + 1) * 128], 128) for n in range(16)])),
        "w_out": f(np.stack([pc(inputs["w_out"][0][:, cb * 512:(cb + 1) * 512], 512) for cb in range(4)])),
        "lnp": f(np.stack([inputs["ln1_g"][0], inputs["ln1_b"][0], inputs["ln2_g"][0], inputs["ln2_b"][0]])),
        "w_router": f(inputs["w_router"][0][:, :NE]),
        "b_router": f(inputs["b_router"][0][None, :NE]),
        "b_upg": f(b_up[:, :, :, 0].transpose(2, 0, 1).reshape(128, NE * 16)),
        "b_upu": f(b_up[:, :, :, 1].transpose(2, 0, 1).reshape(128, NE * 16)),
        "b_down": f(inputs["b_down"][0][:NE]),
        "consts": make_consts(),
    }


def run(inputs, S=4096, NE=32, cores=(0, 1), debug=False, phases="0ABC", trace=False):
    bld = Builder(S, NE, debug=debug, phases=phases)
    first = _prep_inputs(inputs, cores[0], S, NE)
    in_maps = [{k: v for k, v in _prep_inputs(inputs, b, S, NE, shared=first).items() if k in bld.decl} for b in cores]
    res = run_bass_kernel_spmd(bld.nc, in_maps, core_ids=list(range(len(cores))), trace=trace)
    return res, bld


def kernel(**inputs):
    inputs = {k: np.asarray(v) for k, v in inputs.items()}
    res, _ = run(inputs)
    return np.stack([np.asarray(r["out"], dtype=np.float32) for r in res.results], axis=0)
```
